# Optimizing a Trainium2 kernel written in Bass

```python
import math
import jax, jax.numpy as jnp
from jax import lax
import numpy as np

D_MODEL = 2048
BATCH = 8
SEQ = 2048
DEPTH = 2

CTX_LEN = 256
GRID_W = 64
EPS = 1e-6
NEG_INF = -1e30
N_MOD = 6

D_MIX = D_MODEL
DH = 64
DK_A = 128
D_A = D_MIX // 2
H_A = D_A // DK_A
CONV_W = 5
CHUNK = 64
D_B = D_MIX // 4
H_B = D_B // DH
KV_B = 2
WIN = 128
QBLK = 128
ROPE_BASE = 10000.0
D_C = D_MIX - D_A - D_B
H_C = D_C // DH
KH_MAX = 8
KW = 16
NA_QCOLS = 16
NA_KCOLS = 32
N_IN = 3 * D_A + D_A + 4 * H_A + D_B + 2 * KV_B * DH + 3 * D_C
N_GROUPS = 4
E_PER_GROUP = 8
N_EXPERTS = N_GROUPS * E_PER_GROUP
TOP_K = 2
D_EXPERT = 1408
MOE_BLK = 128

kernel_name = "hybrid_dit_gdn_swa_natten_hmoe"


def rmsnorm(x, gain):
    x32 = x.astype(jnp.float32)
    y = x32 * lax.rsqrt(jnp.mean(x32 * x32, -1, keepdims=True) + EPS)
    return (y * gain).astype(x.dtype)


def modulate(h, shift, scale):
    return h * (1 + scale) + shift


def l2norm(u):
    u32 = u.astype(jnp.float32)
    return u32 * lax.rsqrt(jnp.sum(u32 * u32, -1, keepdims=True) + EPS)


def split_projection(p):
    sizes = (3 * D_A, D_A, 4 * H_A, D_B, 2 * KV_B * DH, D_C, 2 * D_C)
    bounds = [sum(sizes[:i + 1]) for i in range(len(sizes) - 1)]
    return jnp.split(p, bounds, axis=-1)


def axial_rope_tables(n_tokens, head_dim):
    t = jnp.arange(n_tokens)
    row = (t // GRID_W).astype(jnp.float32)
    col = (t % GRID_W).astype(jnp.float32)
    half = head_dim // 2
    inv = ROPE_BASE ** (-jnp.arange(0, half, 2, dtype=jnp.float32) / half)
    ang_r = row[:, None] * inv[None]
    ang_c = col[:, None] * inv[None]
    ang = jnp.concatenate([ang_r, ang_r, ang_c, ang_c], -1)
    return jnp.cos(ang), jnp.sin(ang)


def apply_axial_rope(x, cos, sin):
    half = x.shape[-1] // 2

    def rot(u):
        u1, u2 = jnp.split(u, 2, -1)
        return jnp.concatenate([-u2, u1], -1)

    xr = jnp.concatenate([rot(x[..., :half]), rot(x[..., half:])], -1)
    return (x * cos[None, :, None, :] + xr * sin[None, :, None, :]).astype(x.dtype)


def softmax_with_sink(s, sink):
    sk = jnp.broadcast_to(sink, s.shape[:-1] + (1,))
    p = jax.nn.softmax(jnp.concatenate([s, sk], -1), -1)
    return p[..., :-1]


def centred_short_conv(u, w):
    pad = CONV_W // 2
    L = u.shape[1]
    up = jnp.pad(u, ((0, 0), (pad, pad), (0, 0)))
    return sum(up[:, i:i + L] * w[i] for i in range(CONV_W))


def gdn_prepare(qkv, ab, conv_w, a_log, dt_bias):
    B, L, _ = qkv.shape
    qkv = jax.nn.silu(centred_short_conv(qkv, conv_w))
    q, k, v = jnp.split(qkv, 3, -1)
    q = l2norm(q.reshape(B, L, H_A, DK_A))
    k = l2norm(k.reshape(B, L, H_A, DK_A))
    v = v.reshape(B, L, H_A, DK_A)
    ab = ab.astype(jnp.float32).reshape(B, L, 2, 2, H_A)
    g = -jnp.exp(a_log) * jax.nn.softplus(ab[:, :, :, 0] + dt_bias)
    beta = jax.nn.sigmoid(ab[:, :, :, 1])
    return q, k, v, g, beta


def gdn_chunked(q, k, v, g, beta, s0):
    B, L, H, dk = k.shape
    dv = v.shape[-1]
    n = L // CHUNK
    f32 = jnp.float32

    def chunks(u):
        return u.astype(f32).reshape(B, n, CHUNK, H, -1).transpose(0, 3, 1, 2, 4)

    qc, kc, vc = chunks(q * dk ** -0.5), chunks(k), chunks(v)
    gc = g.astype(f32).reshape(B, n, CHUNK, H).transpose(0, 3, 1, 2)
    bc = beta.astype(f32).reshape(B, n, CHUNK, H).transpose(0, 3, 1, 2)
    gcum = jnp.cumsum(gc, -1)
    tril = jnp.tril(jnp.ones((CHUNK, CHUNK), bool))
    strict = jnp.tril(jnp.ones((CHUNK, CHUNK), bool), -1)
    diff = gcum[..., :, None] - gcum[..., None, :]
    decay = jnp.where(tril, jnp.exp(jnp.where(tril, diff, 0.0)), 0.0)
    kb = kc * bc[..., None]
    vb = vc * bc[..., None]
    a = jnp.where(strict, jnp.einsum('bhnid,bhnjd->bhnij', kb, kc) * decay, 0.0)
    eye = jnp.eye(CHUNK, dtype=f32)
    tinv = lax.linalg.triangular_solve(eye + a, jnp.broadcast_to(eye, a.shape),
                                       left_side=True, lower=True, unit_diagonal=True)
    u = tinv @ vb
    w = tinv @ (kb * jnp.exp(gcum)[..., None])
    attn = jnp.einsum('bhnid,bhnjd->bhnij', qc, kc) * decay
    qg = qc * jnp.exp(gcum)[..., None]
    kdec = kc * jnp.exp(gcum[..., -1:] - gcum)[..., None]
    glast = jnp.exp(gcum[..., -1])
    xs = tuple(jnp.moveaxis(t, 2, 0) for t in (u, w, attn, qg, kdec, glast))

    def step(s, inp):
        u_i, w_i, attn_i, qg_i, kdec_i, gl_i = inp
        v_new = u_i - w_i @ s
        o_i = qg_i @ s + attn_i @ v_new
        s = s * gl_i[..., None, None] + jnp.einsum('bhcd,bhce->bhde', kdec_i, v_new)
        return s, o_i

    s_final, o = lax.scan(step, s0.astype(f32), xs)
    o = jnp.moveaxis(o, 0, 2).transpose(0, 2, 3, 1, 4).reshape(B, L, H, dv)
    return o, s_final


def gdn_output(o, z, norm_w):
    B, L = z.shape[:2]
    o = o * lax.rsqrt(jnp.mean(o * o, -1, keepdims=True) + EPS) * norm_w
    zg = jax.nn.silu(z.reshape(B, L, H_A, DK_A).astype(jnp.float32))
    return (o * zg).reshape(B, L, D_A).astype(z.dtype)


def gated_deltanet(qkv_c, ab_c, z_c, qkv_l, ab_l, z_l, conv_w, a_log, dt_bias, norm_w, ctx_out):
    qc, kc, vc, gc, bc = gdn_prepare(qkv_c, ab_c, conv_w, a_log, dt_bias)
    ql, kl, vl, gl, bl = gdn_prepare(qkv_l, ab_l, conv_w, a_log, dt_bias)
    B = ql.shape[0]
    s0 = jnp.zeros((B, H_A, DK_A, DK_A), jnp.float32)
    o_c_sum, o_l_sum = 0.0, 0.0
    for d in range(2):
        seq_c = (qc, kc, vc, gc[:, :, d], bc[:, :, d])
        seq_l = (ql, kl, vl, gl[:, :, d], bl[:, :, d])
        if d == 1:
            seq_c = tuple(jnp.flip(t, 1) for t in seq_c)
            seq_l = tuple(jnp.flip(t, 1) for t in seq_l)
        o_c, s_c = gdn_chunked(*seq_c, s0)
        o_l, _ = gdn_chunked(*seq_l, s_c)
        if d == 1:
            o_c, o_l = jnp.flip(o_c, 1), jnp.flip(o_l, 1)
        o_c_sum = o_c_sum + o_c
        o_l_sum = o_l_sum + o_l
    o_lat = gdn_output(o_l_sum, z_l, norm_w)
    o_ctx = gdn_output(o_c_sum, z_c, norm_w) if ctx_out else None
    return o_ctx, o_lat


def window_gqa(q_c, kv_c, q_l, kv_l, sink, cos, sin, ctx_out):
    B, L = q_l.shape[:2]
    Lc = q_c.shape[1]
    G = H_B // KV_B
    n = L // QBLK
    J = 3 * QBLK
    scale = DH ** -0.5
    f32 = jnp.float32
    k_l, v_l = [u.reshape(B, L, KV_B, DH) for u in jnp.split(kv_l, 2, -1)]
    k_c, v_c = [u.reshape(B, Lc, KV_B, DH) for u in jnp.split(kv_c, 2, -1)]
    q_l = apply_axial_rope(q_l.reshape(B, L, H_B, DH), cos, sin)
    k_l = apply_axial_rope(k_l, cos, sin)
    sink = sink.astype(f32).reshape(KV_B, G, 1, 1)

    def band(u):
        up = jnp.pad(u, ((0, 0), (QBLK, QBLK), (0, 0), (0, 0))).reshape(B, n + 2, QBLK, KV_B, DH)
        return jnp.concatenate([up[:, :-2], up[:, 1:-1], up[:, 2:]], axis=2)

    k_w, v_w = band(k_l), band(v_l)
    qb = q_l.reshape(B, n, QBLK, KV_B, G, DH)
    qi = jnp.arange(QBLK)[:, None]
    kj = jnp.arange(J)[None]
    kpos = (jnp.arange(n) * QBLK)[:, None, None] - QBLK + kj[None]
    in_win = (jnp.abs(kj - QBLK - qi)[None] <= WIN) & (kpos >= 0) & (kpos < L)
    s_w = jnp.einsum('bnqkgd,bnjkd->bnkgqj', qb, k_w).astype(f32) * scale
    s_w = jnp.where(in_win[None, :, None, None], s_w, NEG_INF)
    s_x = jnp.einsum('bnqkgd,bckd->bnkgqc', qb, k_c).astype(f32) * scale
    p = softmax_with_sink(jnp.concatenate([s_w, s_x], -1), sink).astype(v_l.dtype)
    o = (jnp.einsum('bnkgqj,bnjkd->bnqkgd', p[..., :J], v_w)
         + jnp.einsum('bnkgqc,bckd->bnqkgd', p[..., J:], v_c))
    o_lat = o.reshape(B, L, D_B)
    o_ctx = None
    if ctx_out:
        qc = q_c.reshape(B, Lc, KV_B, G, DH)
        s_c = jnp.einsum('bqkgd,bckd->bkgqc', qc, k_c).astype(f32) * scale
        p_c = softmax_with_sink(s_c, sink).astype(v_c.dtype)
        o_ctx = jnp.einsum('bkgqc,bckd->bqkgd', p_c, v_c).reshape(B, Lc, D_B)
    return o_ctx, o_lat


def neighbourhood_attn(q_c, kv_c, q_l, kv_l, rpb, ctx_out):
    B, L = q_l.shape[:2]
    Lc = q_c.shape[1]
    rows = L // GRID_W
    kh = min(KH_MAX, rows)
    ncb = GRID_W // NA_QCOLS
    m = kh * NA_KCOLS
    scale = DH ** -0.5
    f32 = jnp.float32
    k_l, v_l = [u.reshape(B, L, H_C, DH) for u in jnp.split(kv_l, 2, -1)]
    k_c, v_c = [u.reshape(B, Lc, H_C, DH) for u in jnp.split(kv_c, 2, -1)]
    r = jnp.arange(rows)
    row_idx = jnp.clip(r - kh // 2, 0, rows - kh)[:, None] + jnp.arange(kh)[None]
    cb = jnp.arange(ncb)
    col_idx = (jnp.clip(cb * NA_QCOLS - KW // 2, 0, GRID_W - NA_KCOLS)[:, None]
               + jnp.arange(NA_KCOLS)[None])

    def gather_band(u):
        u = u.reshape(B, rows, GRID_W, H_C, DH)[:, row_idx]
        u = u[:, :, :, col_idx]
        return u.transpose(0, 1, 3, 2, 4, 5, 6).reshape(B, rows, ncb, m, H_C, DH)

    k_n, v_n = gather_band(k_l), gather_band(v_l)
    qn = q_l.reshape(B, rows, ncb, NA_QCOLS, H_C, DH)
    q_col = cb[:, None] * NA_QCOLS + jnp.arange(NA_QCOLS)[None]
    q_cs = jnp.clip(q_col - KW // 2, 0, GRID_W - KW)
    key_col = jnp.tile(col_idx, (1, kh))
    key_row = jnp.repeat(row_idx, NA_KCOLS, axis=1)
    in_win = ((key_col[:, None, :] >= q_cs[:, :, None])
              & (key_col[:, None, :] < q_cs[:, :, None] + KW))
    dr = key_row - r[:, None] + (KH_MAX - 1)
    dc = jnp.clip(key_col[:, None, :] - q_col[:, :, None] + (KW - 1), 0, 2 * KW - 2)
    bias = rpb[:, dr[:, None, None, :], dc[None]].astype(f32)
    s_nb = jnp.einsum('brcqhd,brcmhd->bhrcqm', qn, k_n).astype(f32) * scale + bias
    s_nb = jnp.where(in_win, s_nb, NEG_INF)
    s_x = jnp.einsum('brcqhd,bkhd->bhrcqk', qn, k_c).astype(f32) * scale
    p = jax.nn.softmax(jnp.concatenate([s_nb, s_x], -1), -1).astype(v_l.dtype)
    o = (jnp.einsum('bhrcqm,brcmhd->brcqhd', p[..., :m], v_n)
         + jnp.einsum('bhrcqk,bkhd->brcqhd', p[..., m:], v_c))
    o_lat = o.reshape(B, L, D_C)
    o_ctx = None
    if ctx_out:
        qc = q_c.reshape(B, Lc, H_C, DH)
        s_c = jnp.einsum('bqhd,bkhd->bhqk', qc, k_c).astype(f32) * scale
        p_c = jax.nn.softmax(s_c, -1).astype(v_c.dtype)
        o_ctx = jnp.einsum('bhqk,bkhd->bqhd', p_c, v_c).reshape(B, Lc, D_C)
    return o_ctx, o_lat


def hierarchical_moe(h, w_rg, b_rg, w_re, b_re, w_gate, w_up, w_down):
    T, D = h.shape
    f32 = jnp.float32
    g_logits = (h @ w_rg).astype(f32) + b_rg
    g_prob = jax.nn.softmax(g_logits, -1)
    g_sel = jnp.argmax(g_logits, -1)
    e_logits = ((h @ w_re).astype(f32) + b_re).reshape(T, N_GROUPS, E_PER_GROUP)
    e_in = jnp.take_along_axis(e_logits, g_sel[:, None, None], 1)[:, 0]
    top_v, top_i = lax.top_k(e_in, TOP_K)
    wts = jax.nn.softmax(top_v, -1) * jnp.take_along_axis(g_prob, g_sel[:, None], 1)
    expert = g_sel[:, None] * E_PER_GROUP + top_i
    S = T * TOP_K
    e_flat = expert.reshape(S)
    order = jnp.argsort(e_flat)
    e_sorted = e_flat[order]
    tok_sorted = order // TOP_K
    w_sorted = wts.reshape(S)[order]
    counts = jnp.bincount(e_flat, length=N_EXPERTS)
    starts = jnp.cumsum(counts) - counts
    padded = (counts + MOE_BLK - 1) // MOE_BLK * MOE_BLK
    pends = jnp.cumsum(padded)
    pstarts = pends - padded
    dest = pstarts[e_sorted] + jnp.arange(S) - starts[e_sorted]
    n_blk = -(-S // MOE_BLK) + N_EXPERTS
    P = n_blk * MOE_BLK
    buf = jnp.zeros((P, D), h.dtype).at[dest].set(h[tok_sorted])
    blk_e = jnp.minimum(jnp.searchsorted(pends, jnp.arange(n_blk) * MOE_BLK, side='right'), N_EXPERTS - 1)

    def expert_block(args):
        xb, e = args
        return (jax.nn.silu(xb @ w_gate[e]) * (xb @ w_up[e])) @ w_down[e]

    out = lax.map(expert_block, (buf.reshape(n_blk, MOE_BLK, D), blk_e)).reshape(P, D)
    contrib = out[dest] * w_sorted[:, None].astype(out.dtype)
    return jnp.zeros((T, D), out.dtype).at[tok_sorted].add(contrib)


def setup_inputs(seed: int = 0) -> dict:
    key = jax.random.key(seed)
    ks = jax.random.split(key, 26)
    f32 = jnp.float32

    def nrm(k, shape, scale):
        return jax.random.normal(k, shape, f32) * scale

    dt = jnp.exp(jax.random.uniform(ks[10], (DEPTH, 2, H_A), f32, math.log(1e-3), math.log(1e-1)))
    return {
        'x': nrm(ks[0], (BATCH, SEQ, D_MODEL), 1.0),
        'c': nrm(ks[1], (BATCH, D_MODEL), 1.0),
        'ctx': nrm(ks[2], (BATCH, CTX_LEN, D_MODEL), 1.0),
        'c_ctx': nrm(ks[3], (D_MODEL,), 1.0),
        'w_mod': nrm(ks[4], (DEPTH, D_MODEL, N_MOD * D_MODEL), 0.5 * D_MODEL ** -0.5),
        'b_mod': nrm(ks[5], (DEPTH, N_MOD * D_MODEL), 0.02),
        'norm_mix': 1.0 + nrm(ks[6], (DEPTH, D_MODEL), 0.02),
        'norm_ffn': 1.0 + nrm(ks[7], (DEPTH, D_MODEL), 0.02),
        'w_in': nrm(ks[8], (DEPTH, D_MODEL, N_IN), D_MODEL ** -0.5),
        'conv_a': nrm(ks[9], (DEPTH, CONV_W, 3 * D_A), CONV_W ** -0.5),
        'a_log': jnp.log(jax.random.uniform(ks[11], (DEPTH, 2, H_A), f32, 1.0, 16.0)),
        'dt_bias': dt + jnp.log(-jnp.expm1(-dt)),
        'gdn_norm': 1.0 + nrm(ks[12], (DEPTH, DK_A), 0.02),
        'sink_b': nrm(ks[13], (DEPTH, H_B), 1.0),
        'rpb_c': nrm(ks[14], (DEPTH, H_C, 2 * KH_MAX - 1, 2 * KW - 1), 0.5),
        'w_out': nrm(ks[15], (DEPTH, D_MIX, D_MODEL), D_MIX ** -0.5),
        'w_router_group': nrm(ks[16], (DEPTH, D_MODEL, N_GROUPS), D_MODEL ** -0.5),
        'b_router_group': nrm(ks[17], (DEPTH, N_GROUPS), 0.01),
        'w_router_expert': nrm(ks[18], (DEPTH, D_MODEL, N_EXPERTS), D_MODEL ** -0.5),
        'b_router_expert': nrm(ks[19], (DEPTH, N_EXPERTS), 0.01),
        'w_gate': nrm(ks[20], (DEPTH, N_EXPERTS, D_MODEL, D_EXPERT), D_MODEL ** -0.5),
        'w_up': nrm(ks[21], (DEPTH, N_EXPERTS, D_MODEL, D_EXPERT), D_MODEL ** -0.5),
        'w_down': nrm(ks[22], (DEPTH, N_EXPERTS, D_EXPERT, D_MODEL), D_EXPERT ** -0.5),
        'norm_final': 1.0 + nrm(ks[23], (D_MODEL,), 0.02),
    }


def reference(x, c, ctx, c_ctx, w_mod, b_mod, norm_mix, norm_ffn, w_in, conv_a, a_log, dt_bias,
              gdn_norm, sink_b, rpb_c, w_out, w_router_group, b_router_group, w_router_expert,
              b_router_expert, w_gate, w_up, w_down, norm_final):
    B, L, D = x.shape
    Lc = ctx.shape[1]
    cos, sin = axial_rope_tables(L, DH)
    x_l, x_c = x, ctx
    for l in range(DEPTH):
        last = l == DEPTH - 1
        ctx_out = not last
        mod_l = (jax.nn.silu(c) @ w_mod[l] + b_mod[l]).reshape(B, N_MOD, D)
        mod_c = (jax.nn.silu(c_ctx) @ w_mod[l] + b_mod[l]).reshape(1, N_MOD, D)
        h_l = modulate(rmsnorm(x_l, norm_mix[l]), mod_l[:, 0:1], mod_l[:, 1:2])
        h_c = modulate(rmsnorm(x_c, norm_mix[l]), mod_c[:, 0:1], mod_c[:, 1:2])
        qkvA_l, zA_l, abA_l, qB_l, kvB_l, qC_l, kvC_l = split_projection(h_l @ w_in[l])
        qkvA_c, zA_c, abA_c, qB_c, kvB_c, qC_c, kvC_c = split_projection(h_c @ w_in[l])
        oA_c, oA_l = gated_deltanet(qkvA_c, abA_c, zA_c, qkvA_l, abA_l, zA_l,
                                    conv_a[l], a_log[l], dt_bias[l], gdn_norm[l], ctx_out)
        oB_c, oB_l = window_gqa(qB_c, kvB_c, qB_l, kvB_l, sink_b[l], cos, sin, ctx_out)
        oC_c, oC_l = neighbourhood_attn(qC_c, kvC_c, qC_l, kvC_l, rpb_c[l], ctx_out)
        x_l = x_l + mod_l[:, 2:3] * (jnp.concatenate([oA_l, oB_l, oC_l], -1) @ w_out[l])
        if ctx_out:
            x_c = x_c + mod_c[:, 2:3] * (jnp.concatenate([oA_c, oB_c, oC_c], -1) @ w_out[l])
        moe_w = (w_router_group[l], b_router_group[l], w_router_expert[l], b_router_expert[l],
                 w_gate[l], w_up[l], w_down[l])
        f_l = modulate(rmsnorm(x_l, norm_ffn[l]), mod_l[:, 3:4], mod_l[:, 4:5]).reshape(B * L, D)
        if ctx_out:
            f_c = modulate(rmsnorm(x_c, norm_ffn[l]), mod_c[:, 3:4], mod_c[:, 4:5]).reshape(B * Lc, D)
            y = hierarchical_moe(jnp.concatenate([f_c, f_l], 0), *moe_w)
            y_c, y_l = y[:B * Lc], y[B * Lc:]
            x_c = x_c + mod_c[:, 5:6] * y_c.reshape(B, Lc, D)
        else:
            y_l = hierarchical_moe(f_l, *moe_w)
        x_l = x_l + mod_l[:, 5:6] * y_l.reshape(B, L, D)
    return rmsnorm(x_l, norm_final)
```

```python
import numpy as np
from contextlib import ExitStack
import concourse.bass as bass
import concourse.mybir as mybir
from concourse.bass_utils import run_bass_kernel_spmd

F32 = mybir.dt.float32
BF16 = mybir.dt.bfloat16
ALU = mybir.AluOpType
AF = mybir.ActivationFunctionType
AX = mybir.AxisListType
class Buf:
    __slots__ = ("name", "w", "r", "dsem", "dcnt", "excl")
    PSUM_PREFIX = ("pm", "npt", "prt", "pp", "g_pb", "g_pk", "a_pS", "a_pO", "a_pT", "c_pS", "c_pO", "c_pT", "pg", "o_pp", "m_pG", "m_pU", "m_pY")

    def __init__(self, name=""):
        self.name = name
        self.w = []
        self.r = []
        self.dsem = None
        self.dcnt = 0
        self.excl = name.endswith("_p") or name.startswith(self.PSUM_PREFIX)


class KB:
    ENG = ("pe", "dve", "act", "pool", "sp")

    def __init__(self, nc):
        self.nc = nc
        self.eng = {"pe": nc.tensor, "dve": nc.vector, "act": nc.scalar,
                    "pool": nc.gpsimd, "sp": nc.sync}
        self.sems = {}
        self.cnt = {}
        for e in self.ENG:
            self.sems[e] = nc.alloc_semaphore("s_" + e)
            self.cnt[e] = 0
        self.known = {e: {} for e in self.ENG}
        self.ndsem = 0
        self.free_dsems = []
        self.dsem_bufs = []
        self.n_inst = 0
        self.n_wait = 0

    def _dsem(self, buf):
        if buf.dsem is None:
            if self.free_dsems:
                buf.dsem = self.free_dsems.pop()
            else:
                key = "d%d" % self.ndsem
                self.ndsem += 1
                self.sems[key] = self.nc.alloc_semaphore(key)
                self.cnt[key] = 0
                buf.dsem = key
            buf.dcnt = self.cnt[buf.dsem]
            self.dsem_bufs.append(buf)
        return buf.dsem

    def _wait(self, e, key, val):
        if self.known[e].get(key, 0) >= val:
            return
        self.eng[e].wait_ge(self.sems[key], val)
        self.known[e][key] = val
        self.n_wait += 1

    def _deps(self, e, reads, writes, skip_keys=()):
        evs = {}
        for b in reads:
            for (k, v) in b.w:
                if k in skip_keys:
                    continue
                evs[k] = max(evs.get(k, 0), v)
            if b.excl:
                for (k, v) in b.r:
                    if k != e:
                        evs[k] = max(evs.get(k, 0), v)
        for b in writes:
            for (k, v) in b.w:
                if k == e or k in skip_keys:
                    continue
                evs[k] = max(evs.get(k, 0), v)
            for (k, v) in b.r:
                if k == e or k in skip_keys:
                    continue
                evs[k] = max(evs.get(k, 0), v)
        for k, v in evs.items():
            if k[0] == "d" and k != "dve":
                v = self.cnt[k]
            self._wait(e, k, v)

    def _record(self, ev, reads, writes):
        k, v = ev
        for b in writes:
            b.w = [ev]
            b.r = []
        for b in reads:
            b.r = [(kk, vv) for (kk, vv) in b.r if kk != k] + [ev]

    def op(self, e, fn, reads=(), writes=()):
        self._deps(e, reads, writes)
        ins = fn()
        self.cnt[e] += 1
        ins.then_inc(self.sems[e], 1)
        self._record((e, self.cnt[e]), reads, writes)
        self.n_inst += 1
        return ins

    def dma(self, q, out, in_, reads=(), writes=(), sem_buf=None, part=False, **kw):
        sb = sem_buf if sem_buf is not None else (writes[0] if writes else reads[0])
        key = self._dsem(sb)
        skip = (key,) if part else ()
        self._deps(q, reads, writes, skip_keys=skip)
        ins = self.eng[q].dma_start(out=out, in_=in_, **kw)
        self.cnt[key] += 16
        ins.then_inc(self.sems[key], 16)
        ev = (key, self.cnt[key])
        if part:
            for b in writes:
                b.w = [x for x in b.w if x[0] != key] + [ev]
                b.r = []
            for b in reads:
                b.r = [(kk, vv) for (kk, vv) in b.r if kk != key] + [ev]
        else:
            self._record(ev, reads, writes)
        self.n_inst += 1
        return ins

    def barrier(self):
        for e in self.ENG:
            for k in self.sems:
                if k == e:
                    continue
                if self.cnt[k] > 0:
                    self._wait(e, k, self.cnt[k])

    def release_dsems(self):
        for b in self.dsem_bufs:
            if b.dsem is not None:
                self.free_dsems.append(b.dsem)
                b.dsem = None
        self.dsem_bufs = []

    def finish(self):
        for k in self.sems:
            if k != "sp" and self.cnt[k] > 0:
                self._wait("sp", k, self.cnt[k])


D = 2048
DC = 16
T = 2304
LC = 256
NTC = 18
N_IN = 6432
NT5 = [(0, 256), (256, 768), (768, 1280), (1280, 1792), (1792, 2304)]
EPS = 1e-6
C_QKV = 0
C_Z = 3072
C_AB = 4096
C_QB = 4128
C_KVB = 4640
C_QC = 4896
C_KVC = 5408
NE = 32
DE = 1408
FC = 11
DK_SCALE = 128 ** -0.5

K_ID, K_ONES, K_TRI0, K_TRI1, K_MP0, K_MP1, K_ST0, K_ST1, K_SL0, K_SL1 = [i * 128 for i in range(10)]
K_WPREV = 1280
K_WNEXT = 1408
K_CM = 1536
K_ROT = 1600
K_ER = 1664
NCST = 1664


def make_consts():
    c = np.zeros((128, NCST), np.float32)
    p = np.arange(128)[:, None]
    i = np.arange(128)[None]
    c[:, K_ID:K_ID + 128] = (p == i)
    c[:, K_ONES:K_ONES + 128] = 1.0
    c[:, K_TRI0:K_TRI0 + 128] = (p <= i)
    c[:, K_TRI1:K_TRI1 + 128] = (p >= i)
    c[:, K_MP0:K_MP0 + 128] = np.where(p >= i, 0.0, 30000.0)
    c[:, K_MP1:K_MP1 + 128] = np.where(p <= i, 0.0, 30000.0)
    c[:, K_ST0:K_ST0 + 128] = (p > i)
    c[:, K_ST1:K_ST1 + 128] = (p < i)
    c[:, K_SL0:K_SL0 + 128] = (p == 127)
    c[:, K_SL1:K_SL1 + 128] = (p == 0)
    c[:, K_WPREV:K_WPREV + 128] = np.where(p >= i, 0.0, -30000.0)
    c[:, K_WNEXT:K_WNEXT + 128] = np.where(p <= i, 0.0, -30000.0)
    kcol = (np.arange(128) % 64)[:, None]
    qcol = np.arange(64)[None]
    qcs = np.clip(qcol - 8, 0, 48)
    c[:, K_CM:K_CM + 64] = np.where((kcol >= qcs) & (kcol < qcs + 16), 0.0, -30000.0)
    R = np.zeros((64, 64), np.float32)
    for base in (0, 32):
        for d in range(16):
            R[base + d, base + d + 16] = -1.0
            R[base + d + 16, base + d] = 1.0
    c[0:64, K_ROT:K_ROT + 64] = R.T
    return c


def rope_tables():
    t = np.arange(2048)
    row = (t // 64).astype(np.float32)
    col = (t % 64).astype(np.float32)
    inv = (10000.0 ** (-np.arange(0, 32, 2, dtype=np.float32) / 32)).astype(np.float32)
    ang_r = row[:, None] * inv[None]
    ang_c = col[:, None] * inv[None]
    ang = np.concatenate([ang_r, ang_r, ang_c, ang_c], -1).astype(np.float32)
    return np.ascontiguousarray(np.cos(ang).T.astype(np.float32)), np.ascontiguousarray(np.sin(ang).T.astype(np.float32))


class Ctx:
    pass


def build(debug=False, stop=None, depth=2, moe_decl=(2, NE), ne_run=NE):
    nc = bass.Bass("TRN2", target_bir_lowering=False)
    kb = KB(nc)
    g = Ctx()
    dbg_kind = "ExternalOutput" if debug else "Internal"

    def din(name, shape, dt=F32):
        return nc.dram_tensor(name, list(shape), dt, kind="ExternalInput").ap()

    def dscr(name, shape, dt=F32):
        return nc.dram_tensor(name, list(shape), dt, kind=dbg_kind).ap()

    x_in = din("x", [2048, D])
    ctx_in = din("ctx", [LC, D])
    cvec = din("cvec", [32, 128])
    w_mod = din("w_mod", [2, D, 6 * D])
    b_mod = din("b_mod", [2, 96, 128])
    norm_mix = din("norm_mix", [2, 16, 128])
    norm_ffn = din("norm_ffn", [2, 16, 128])
    norm_final = din("norm_final", [1, D])
    w_in = din("w_in", [2, D, N_IN])
    conv_a = din("conv_a", [2, 120, 128])
    gdn_cols = din("gdn_cols", [2, 2, 32])
    gdn_norm = din("gdn_norm", [2, 1, 128])
    sink_b = din("sink_b", [2, 1, 8])
    rpb_pad = din("rpb_pad", [2, 8 * 15, 128])
    w_out = din("w_out", [2, D, D])
    w_r = din("w_r", [2, D, 36])
    b_r = din("b_r", [2, 1, 36])
    need_moe = stop is None or stop in ("moe", "final")
    if need_moe:
        w_gate = din("w_gate", [moe_decl[0], moe_decl[1], D, DE])
        w_up = din("w_up", [moe_decl[0], moe_decl[1], D, DE])
        w_down = din("w_down", [moe_decl[0], moe_decl[1], DE, D])
    cst_in = din("cst", [128, NCST])
    ropec = din("ropec", [64, 2048])
    ropes = din("ropes", [64, 2048])
    out = nc.dram_tensor("out", [2048, D], F32, kind="ExternalOutput").ap()

    xs = [dscr("xres%d" % i, [T, D]) for i in range(4)]
    qkvT = dscr("qkvT", [24, 128, T])
    z_tok = dscr("z_tok", [T, 1024])
    ab_tok = dscr("ab_tok", [T, 32])
    qBT = dscr("qBT", [8, 64, T], BF16)
    kBT = dscr("kBT", [2, 64, T], BF16)
    vB = dscr("vB", [T, 128], BF16)
    qCT = dscr("qCT", [8, 64, T], BF16)
    kCT = dscr("kCT", [8, 64, T], BF16)
    vC = dscr("vC", [T, 512], BF16)
    OTd = dscr("OTd", [16, 128, T], BF16)
    Bxs = [Buf("xres%d" % i) for i in range(4)]
    BqkvT, Bz, Bab, BqBT, BkBT, BvB, BqCT, BkCT, BvC, BOT = [Buf(n) for n in
        "qkvT z ab qBT kBT vB qCT kCT vC OT".split()]
    Bin = Buf("inputs")

    es_all = ExitStack()

    uid = [0]

    def sb(es, name, shape, dt=F32):
        uid[0] += 1
        return es.enter_context(nc.sbuf_tensor("%s_s%d" % (name, uid[0]), list(shape), dt))

    def ps(es, name, shape, dt=F32):
        uid[0] += 1
        return es.enter_context(nc.psum_tensor("%s_p%d" % (name, uid[0]), list(shape), dt))

    V, S_, P_, PE = nc.vector, nc.scalar, nc.gpsimd, nc.tensor

    cst = sb(es_all, "cst", [128, NCST]); Bcst = Buf("cst")
    kb.dma("sp", cst[:], cst_in, writes=[Bcst])
    IDENT = cst[:, K_ID:K_ID + 128]
    ONES = cst[:, K_ONES:K_ONES + 128]
    identb = sb(es_all, "identb", [128, 128], BF16); Bidb = Buf("identb")
    kb.op("dve", lambda: V.tensor_copy(identb[:], IDENT), reads=[Bcst], writes=[Bidb])
    sc = sb(es_all, "sc", [128, 16, 2], BF16); Bsc = Buf("sc")
    modT = sb(es_all, "modT", [128, 96, 2]); Bmod = Buf("modT")
    modA = sb(es_all, "modA", [128, 2, 16, 2]); BmodA = Buf("modA")

    def phase_end():
        kb.barrier()
        kb.release_dsems()

    def load_fm(es, src_ap, n, dst_ap, dstbuf, name):
        with ExitStack() as e2:
            tmp = sb(e2, name + "_t", [128, 128]); Bt = Buf(name + "_t")
            pt = ps(e2, name + "_p", [128, 128]); Bp = Buf(name + "_p")
            kb.dma("sp", tmp[0:n, :], src_ap, reads=[Bin], writes=[Bt])
            kb.op("pe", lambda: PE.transpose(pt[:, 0:n], tmp[0:n, :], IDENT[0:n, 0:n]), reads=[Bt, Bcst], writes=[Bp])
            kb.op("act", lambda: S_.copy(dst_ap, pt[:, 0:n]), reads=[Bp], writes=[dstbuf])
            kb.barrier()

    with ExitStack() as es:
        ctmp = sb(es, "ctmp", [128, 32]); Bct = Buf("ctmp")
        load_fm(es, cvec, 32, ctmp[:], Bct, "cv")
        sg = sb(es, "sg", [128, 32]); Bsg = Buf("sg")
        kb.op("act", lambda: S_.activation(sg[:], ctmp[:], AF.Sigmoid), reads=[Bct], writes=[Bsg])
        kb.op("dve", lambda: V.tensor_tensor(sc[:].rearrange("p c j -> p j c"), ctmp[:].rearrange("p (j c) -> p j c", j=2),
                                             sg[:].rearrange("p (j c) -> p j c", j=2), ALU.mult),
              reads=[Bct, Bsg], writes=[Bsc])
        phase_end()

    def phase_mod(l):
        with ExitStack() as es:
            wbufs = [sb(es, "wm%d" % i, [128, 6 * D], BF16) for i in range(2)]
            Bw = [Buf("wm%d" % i) for i in range(2)]
            pm = ps(es, "pm", [128, 512]); Bpm = Buf("pm")
            bm = sb(es, "bm", [128, 96]); Bbm = Buf("bm")
            nm = sb(es, "nm", [128, 2, 16]); Bnm = Buf("nm")
            load_fm(es, b_mod[l], 96, bm[:], Bbm, "bmod")
            load_fm(es, norm_mix[l], 16, nm[:, 0, :], Bnm, "nmix")
            load_fm(es, norm_ffn[l], 16, nm[:, 1, :], Bnm, "nffn")
            for dc in range(DC):
                kb.dma("pool", wbufs[dc % 2][:], w_mod[l, dc * 128:(dc + 1) * 128, :], reads=[Bin], writes=[Bw[dc % 2]])
                for n in range(96):
                    kb.op("pe", lambda n=n, dc=dc: PE.matmul(pm[:, 2 * n:2 * n + 2], wbufs[dc % 2][:, n * 128:(n + 1) * 128],
                                                            sc[:, dc, :], start=(dc == 0 and n == 0), stop=(dc == DC - 1 and n == 95), skip_group_check=True),
                          reads=[Bw[dc % 2], Bsc], writes=[Bpm])
            kb.op("dve", lambda: V.tensor_tensor(modT[:], pm[:, 0:192].rearrange("p (n j) -> p n j", j=2),
                                                 bm[:].unsqueeze(2).to_broadcast([128, 96, 2]), ALU.add),
                  reads=[Bpm, Bbm], writes=[Bmod])
            for w, s0 in ((0, 16), (1, 64)):
                kb.op("dve", lambda w=w, s0=s0: V.scalar_tensor_tensor(
                    out=modA[:, w], in0=modT[:, s0:s0 + 16, :], scalar=1.0,
                    in1=nm[:, w, :].unsqueeze(2).to_broadcast([128, 16, 2]), op0=ALU.add, op1=ALU.mult),
                    reads=[Bmod, Bnm], writes=[BmodA])
            phase_end()

    def x_src(which, c):
        if which < 0:
            if c < 2:
                return ctx_in[c * 128:(c + 1) * 128, :], Bin
            return x_in[(c - 2) * 128:(c - 1) * 128, :], Bin
        return xs[which][c * 128:(c + 1) * 128, :], Bxs[which]

    def phase_norm(l, w, which, hT, BhT, router=None, c_lo=0):
        sh0 = 0 if w == 0 else 48
        with ExitStack() as es:
            xt = [sb(es, "nx%d" % i, [128, D]) for i in range(2)]; Bxt = [Buf("nx%d" % i) for i in range(2)]
            sq = sb(es, "nsq", [128, D]); Bsq = Buf("nsq")
            ss = sb(es, "nss", [128, 2]); Bss = Buf("nss")
            pt = [ps(es, "npt%d" % i, [128, 4, 128]) for i in range(4)]; Bpt = [Buf("npt%d" % i) for i in range(4)]
            f32t = None
            if router is not None:
                f32t = sb(es, "f32t", [128, 16, 128]); Bf32 = Buf("f32t")
                prt = ps(es, "prt", [128, 512]); Bprt = Buf("prt")
                wr_sb, Bwr, rl_sb, Brl = router
            for c in range(c_lo, NTC):
                j = 0 if c >= 2 else 1
                src, Bsrc = x_src(which, c)
                xb = xt[c % 2]; Bx = Bxt[c % 2]
                kb.dma("sp", xb[:], src, reads=[Bsrc], writes=[Bx])
                kb.op("act", lambda xb=xb: S_.activation(sq[:], xb[:], AF.Square, accum_out=ss[:, 0:1]), reads=[Bx], writes=[Bsq, Bss])
                kb.op("act", lambda: S_.activation(ss[:, 1:2], ss[:, 0:1], AF.Sqrt, bias=EPS, scale=1.0 / D), reads=[Bss], writes=[Bss])
                kb.op("dve", lambda: V.reciprocal(ss[:, 1:2], ss[:, 1:2]), reads=[Bss], writes=[Bss])
                kb.op("dve", lambda xb=xb: V.tensor_scalar(xb[:], xb[:], ss[:, 1:2], None, ALU.mult), reads=[Bx, Bss], writes=[Bx])
                for q4 in range(4):
                    for cc in range(4):
                        dc = q4 * 4 + cc
                        kb.op("pe", lambda xb=xb, dc=dc, q4=q4, cc=cc: PE.transpose(pt[q4][:, cc, :], xb[:, dc * 128:(dc + 1) * 128], IDENT),
                              reads=[Bx, Bcst], writes=[Bpt[q4]])
                    for cc in range(4):
                        dc = q4 * 4 + cc
                        eng = "act" if q4 % 2 == 0 else "dve"
                        if router is None:
                            dst = hT[:, dc, c * 128:(c + 1) * 128]; wb = [BhT]
                        else:
                            dst = f32t[:, dc, :]; wb = [Bf32]
                        if eng == "act":
                            kb.op("act", lambda dst=dst, q4=q4, cc=cc, dc=dc, j=j: S_.activation(
                                dst, pt[q4][:, cc, :], AF.Identity, bias=modT[:, sh0 + dc, j:j + 1], scale=modA[:, w, dc, j:j + 1]),
                                reads=[Bpt[q4], Bmod, BmodA], writes=wb)
                        else:
                            kb.op("dve", lambda dst=dst, q4=q4, cc=cc, dc=dc, j=j: V.tensor_scalar(
                                dst, pt[q4][:, cc, :], modA[:, w, dc, j:j + 1], modT[:, sh0 + dc, j:j + 1], ALU.mult, ALU.add),
                                reads=[Bpt[q4], Bmod, BmodA], writes=wb)
                if router is not None:
                    kb.op("pool", lambda c=c: P_.tensor_copy(hT[:, :, c * 128:(c + 1) * 128], f32t[:]), reads=[Bf32], writes=[BhT])
                    for dc in range(DC):
                        kb.op("pe", lambda dc=dc: PE.matmul(prt[:, 0:36], f32t[:, dc, :], wr_sb[:, dc, :], start=(dc == 0), stop=(dc == DC - 1)),
                              reads=[Bf32, Bwr], writes=[Bprt])
                    kb.op("act", lambda c=c: S_.copy(rl_sb[:, c, :], prt[:, 0:36]), reads=[Bprt], writes=[Brl])
            phase_end()


    def phase_proj(l, hT, BhT):
        w_v = w_in[l].rearrange("(dc p) n -> p dc n", p=128)
        with ExitStack() as es:
            wts = [sb(es, "pw%d" % i, [128, 16, 512], BF16) for i in range(2)]; Bw = [Buf("pw%d" % i) for i in range(2)]
            stg = [sb(es, "pst%d" % i, [128, T]) for i in range(2)]; Bst = [Buf("pst%d" % i) for i in range(2)]
            stb = [sb(es, "psb%d" % i, [64, T], BF16) for i in range(2)]; Bsb = [Buf("psb%d" % i) for i in range(2)]
            stt = [sb(es, "ptt%d" % i, [128, 512]) for i in range(2)]; Btt = [Buf("ptt%d" % i) for i in range(2)]
            stv = [sb(es, "ptv%d" % i, [128, 512], BF16) for i in range(2)]; Btv = [Buf("ptv%d" % i) for i in range(2)]
            xf = sb(es, "pxf", [64, 512]); Bxf = Buf("pxf")
            t1 = sb(es, "pt1", [64, 512]); Bt1 = Buf("pt1")
            rc = sb(es, "prc", [64, 2048]); rs = sb(es, "prs", [64, 2048]); Brope = Buf("rope")
            pp = [ps(es, "pp%d" % i, [128, 512]) for i in range(3)]; Bpp = [Buf("pp%d" % i) for i in range(3)]
            pr = ps(es, "ppr", [128, 512]); Bpr = Buf("ppr")
            kb.dma("sp", rc[:], ropec, reads=[Bin], writes=[Brope])
            kb.dma("sp", rs[:], ropes, reads=[Bin], writes=[Brope])
            cnt = {"w": 0, "p": 0, "s": 0, "b": 0, "t": 0, "v": 0}

            def load_w(col0, ncols):
                i = cnt["w"] % 2; cnt["w"] += 1
                kb.dma("pool", wts[i][:, :, 0:ncols], w_v[:, :, col0:col0 + ncols], reads=[Bin], writes=[Bw[i]])
                return wts[i], Bw[i]

            def acc_fm(wt, Bwt, off, m, n0, n1):
                i = cnt["p"] % 3; cnt["p"] += 1
                for dc in range(DC):
                    kb.op("pe", lambda dc=dc: PE.matmul(pp[i][0:m, 0:n1 - n0], wt[:, dc, off:off + m], hT[:, dc, n0:n1],
                                                        start=(dc == 0), stop=(dc == DC - 1)),
                          reads=[Bwt, BhT], writes=[Bpp[i]])
                return pp[i], Bpp[i]

            def acc_tm(wt, Bwt, off, n, c):
                i = cnt["p"] % 3; cnt["p"] += 1
                for dc in range(DC):
                    kb.op("pe", lambda dc=dc: PE.matmul(pp[i][:, 0:n], hT[:, dc, c * 128:(c + 1) * 128], wt[:, dc, off:off + n],
                                                        start=(dc == 0), stop=(dc == DC - 1)),
                          reads=[Bwt, BhT], writes=[Bpp[i]])
                return pp[i], Bpp[i]

            for blk in range(6):
                wt, Bwt = load_w(C_QKV + blk * 512, 512)
                for cc in range(4):
                    ch = blk * 4 + cc
                    si = cnt["s"] % 2; cnt["s"] += 1
                    for ti, (n0, n1) in enumerate(NT5):
                        p, Bp = acc_fm(wt, Bwt, cc * 128, 128, n0, n1)
                        if ti % 2 == 0:
                            kb.op("act", lambda p=p, si=si, n0=n0, n1=n1: S_.copy(stg[si][:, n0:n1], p[:, 0:n1 - n0]), reads=[Bp], writes=[Bst[si]])
                        else:
                            kb.op("dve", lambda p=p, si=si, n0=n0, n1=n1: V.tensor_copy(stg[si][:, n0:n1], p[:, 0:n1 - n0]), reads=[Bp], writes=[Bst[si]])
                    kb.dma("sp", qkvT[ch], stg[si][:], reads=[Bst[si]], writes=[BqkvT])

            def heads(col0, nh, dst, Bdst, scale, rope):
                for h0 in range(0, nh, 8):
                    nhh = min(8, nh - h0)
                    wt, Bwt = load_w(col0 + h0 * 64, nhh * 64)
                    for hh in range(nhh):
                        h = h0 + hh
                        bi = cnt["b"] % 2; cnt["b"] += 1
                        for ti, (n0, n1) in enumerate(NT5):
                            p, Bp = acc_fm(wt, Bwt, hh * 64, 64, n0, n1)
                            n = n1 - n0
                            if rope and ti > 0:
                                l0 = n0 - LC
                                kb.op("act", lambda p=p, n=n: S_.mul(xf[:, 0:n], p[0:64, 0:n], scale), reads=[Bp], writes=[Bxf])
                                kb.op("pe", lambda n=n: PE.matmul(pr[0:64, 0:n], cst[0:64, K_ROT:K_ROT + 64], xf[:, 0:n], start=True, stop=True),
                                      reads=[Bcst, Bxf], writes=[Bpr])
                                kb.op("dve", lambda n=n, l0=l0: V.tensor_tensor(t1[:, 0:n], xf[:, 0:n], rc[:, l0:l0 + n], ALU.mult),
                                      reads=[Bxf, Brope], writes=[Bt1])
                                kb.op("dve", lambda n=n, l0=l0: V.tensor_tensor(xf[:, 0:n], pr[0:64, 0:n], rs[:, l0:l0 + n], ALU.mult),
                                      reads=[Bpr, Brope], writes=[Bxf])
                                kb.op("dve", lambda n=n, n0=n0, bi=bi: V.tensor_tensor(stb[bi][:, n0:n0 + n], t1[:, 0:n], xf[:, 0:n], ALU.add),
                                      reads=[Bt1, Bxf], writes=[Bsb[bi]])
                            else:
                                kb.op("act", lambda p=p, n=n, n0=n0, bi=bi: S_.mul(stb[bi][:, n0:n0 + n], p[0:64, 0:n], scale),
                                      reads=[Bp], writes=[Bsb[bi]])
                        kb.dma("sp", dst[h], stb[bi][:], reads=[Bsb[bi]], writes=[Bdst])
            heads(C_QB, 8, qBT, BqBT, 0.125, True)
            heads(C_KVB, 2, kBT, BkBT, 1.0, True)
            heads(C_QC, 8, qCT, BqCT, 0.125, False)
            heads(C_KVC, 8, kCT, BkCT, 1.0, False)

            def tok(col0, ncols, dst, Bdst, dcol0, bf):
                wt, Bwt = load_w(col0, ncols)
                for c in range(NTC):
                    p, Bp = acc_tm(wt, Bwt, 0, ncols, c)
                    if bf:
                        i = cnt["v"] % 2; cnt["v"] += 1
                        kb.op("act", lambda p=p, i=i: S_.copy(stv[i][:, 0:ncols], p[:, 0:ncols]), reads=[Bp], writes=[Btv[i]])
                        kb.dma("sp", dst[c * 128:(c + 1) * 128, dcol0:dcol0 + ncols], stv[i][:, 0:ncols], reads=[Btv[i]], writes=[Bdst])
                    else:
                        i = cnt["t"] % 2; cnt["t"] += 1
                        kb.op("act", lambda p=p, i=i: S_.copy(stt[i][:, 0:ncols], p[:, 0:ncols]), reads=[Bp], writes=[Btt[i]])
                        kb.dma("sp", dst[c * 128:(c + 1) * 128, dcol0:dcol0 + ncols], stt[i][:, 0:ncols], reads=[Btt[i]], writes=[Bdst])
            tok(C_Z, 512, z_tok, Bz, 0, False)
            tok(C_Z + 512, 512, z_tok, Bz, 512, False)
            tok(C_AB, 32, ab_tok, Bab, 0, False)
            tok(C_KVB + 128, 128, vB, BvB, 0, True)
            tok(C_KVC + 512, 512, vC, BvC, 0, True)
            phase_end()

    def bcast_rows(src, nparts=128):
        return bass.AP(src.tensor, src.offset, [[0, nparts]] + [list(x) for x in src.ap])

    def phase_gdn(l):
        with ExitStack() as es:
            ab = sb(es, "g_ab", [128, 18, 32]); Bab_s = Buf("g_ab")
            cols = sb(es, "g_cols", [128, 2, 32]); Bcols = Buf("g_cols")
            convT = sb(es, "g_conv", [128, 120]); Bconv = Buf("g_conv")
            nwb = sb(es, "g_nwb", [128, 128]); Bnwb = Buf("g_nwb")
            load_fm(es, conv_a[l], 120, convT[:], Bconv, "conv")
            kb.dma("sp", ab[:], ab_tok.rearrange("(c p) n -> p c n", p=128), reads=[Bab], writes=[Bab_s])
            kb.dma("sp", cols[:], bcast_rows(gdn_cols[l]), reads=[Bin], writes=[Bcols])
            kb.dma("sp", nwb[:], bcast_rows(gdn_norm[l, 0]), reads=[Bin], writes=[Bnwb])
            G = sb(es, "g_G", [128, 18, 32]); BG = Buf("g_G")
            BT = sb(es, "g_BT", [128, 18, 32]); BBT = Buf("g_BT")
            GC = sb(es, "g_GC", [128, 2, 18, 8]); BGC = Buf("g_GC")
            GLOG = sb(es, "g_GLOG", [128, 2, 18, 8]); BGLOG = Buf("g_GLOG")
            EG = sb(es, "g_EG", [128, 2, 18, 8]); BEG_ = Buf("g_EG")
            GL = sb(es, "g_GL", [128, 2, 18, 8]); BGL = Buf("g_GL")
            KD = sb(es, "g_KD", [128, 2, 18, 8]); BKD = Buf("g_KD")
            NB = sb(es, "g_NB", [128, 2, 18, 8]); BNB = Buf("g_NB")
            BE = sb(es, "g_BE", [128, 2, 18, 8]); BBE = Buf("g_BE")
            NBEG = sb(es, "g_NBEG", [128, 2, 18, 8]); BNBEG = Buf("g_NBEG")
            pbig = [ps(es, "g_pb%d" % i, [128, 512]) for i in range(2)]; Bpbig = [Buf("g_pb%d" % i) for i in range(2)]
            kb.op("act", lambda: S_.activation(BT[:], ab[:], AF.Sigmoid), reads=[Bab_s], writes=[BBT])
            kb.op("dve", lambda: V.tensor_tensor(G[:], ab[:], cols[:, 0:1, :].to_broadcast([128, 18, 32]), ALU.add), reads=[Bab_s, Bcols], writes=[BG])
            kb.op("act", lambda: S_.activation(G[:], G[:], AF.Exp), reads=[BG], writes=[BG])
            kb.op("act", lambda: S_.activation(G[:], G[:], AF.Ln, bias=1.0, scale=1.0), reads=[BG], writes=[BG])
            kb.op("act", lambda: S_.activation(cols[:, 1, :], cols[:, 1, :], AF.Exp), reads=[Bcols], writes=[Bcols])
            kb.op("dve", lambda: V.scalar_tensor_tensor(out=G[:], in0=G[:], scalar=-1.0, in1=cols[:, 1:2, :].to_broadcast([128, 18, 32]),
                                                        op0=ALU.mult, op1=ALU.mult), reads=[BG, Bcols], writes=[BG])
            for d in range(2):
                tri = cst[:, K_TRI0:K_TRI0 + 128] if d == 0 else cst[:, K_TRI1:K_TRI1 + 128]
                sel = cst[:, K_SL0:K_SL0 + 128] if d == 0 else cst[:, K_SL1:K_SL1 + 128]
                kb.op("pe", lambda d=d, tri=tri: PE.matmul(pbig[0][:, 0:144].rearrange("p (c h) -> p c h", h=8), tri, G[:, :, d * 16:d * 16 + 8], start=True, stop=True),
                      reads=[Bcst, BG], writes=[Bpbig[0]])
                kb.op("act", lambda d=d: S_.copy(GC[:, d], pbig[0][:, 0:144].rearrange("p (c h) -> p c h", h=8)), reads=[Bpbig[0]], writes=[BGC])
                kb.op("pe", lambda d=d, sel=sel: PE.matmul(pbig[1][:, 0:144], sel, GC[:, d].rearrange("p c h -> p (c h)"), start=True, stop=True),
                      reads=[Bcst, BGC], writes=[Bpbig[1]])
                kb.op("act", lambda d=d: S_.copy(GLOG[:, d].rearrange("p c h -> p (c h)"), pbig[1][:, 0:144]), reads=[Bpbig[1]], writes=[BGLOG])
                kb.op("dve", lambda d=d: V.tensor_copy(BE[:, d], BT[:, :, d * 16 + 8:d * 16 + 16]), reads=[BBT], writes=[BBE])
            kb.op("act", lambda: S_.activation(EG[:], GC[:], AF.Exp), reads=[BGC], writes=[BEG_])
            kb.op("act", lambda: S_.activation(GL[:], GLOG[:], AF.Exp), reads=[BGLOG], writes=[BGL])
            kb.op("dve", lambda: V.tensor_tensor(KD[:], GLOG[:], GC[:], ALU.subtract), reads=[BGLOG, BGC], writes=[BKD])
            kb.op("act", lambda: S_.activation(KD[:], KD[:], AF.Exp), reads=[BKD], writes=[BKD])
            kb.op("dve", lambda: V.tensor_scalar(NB[:], BE[:], -1.0, None, ALU.mult), reads=[BBE], writes=[BNB])
            kb.op("dve", lambda: V.tensor_tensor(NBEG[:], NB[:], EG[:], ALU.mult), reads=[BNB, BEG_], writes=[BNBEG])

            raw = [sb(es, "g_raw%d" % i, [128, 2312]) for i in range(3)]; Braw = [Buf("g_raw%d" % i) for i in range(3)]
            cv_ = [sb(es, "g_cv%d" % i, [128, T]) for i in range(3)]; Bcv = [Buf("g_cv%d" % i) for i in range(3)]
            ktok = sb(es, "g_ktok", [128, 18, 128]); Bktok = Buf("g_ktok")
            vtok = sb(es, "g_vtok", [128, 18, 128]); Bvtok = Buf("g_vtok")
            osum = sb(es, "g_osum", [128, 18, 128]); Bosum = Buf("g_osum")
            zt = sb(es, "g_zt", [128, 18, 128]); Bzt = Buf("g_zt")
            ssq = sb(es, "g_ssq", [128, 18]); Bssq = Buf("g_ssq")
            otb = sb(es, "g_otb", [128, T], BF16); Botb = Buf("g_otb")
            tmp5 = sb(es, "g_tmp5", [128, 512]); Btmp5 = Buf("g_tmp5")
            Sst = [sb(es, "g_S%d" % i, [128, 128]) for i in range(2)]; BS = [Buf("g_S%d" % i) for i in range(2)]
            G_W = 2
            NSET = 2 * G_W
            names = "DG TM DMS AN ATT TT PA PB QA QB KDEC VB".split()
            sets = []
            for si in range(NSET):
                d_ = {}
                for nm_ in names:
                    d_[nm_] = (sb(es, "g_%s%d" % (nm_, si), [128, 128]), Buf("g_%s%d" % (nm_, si)))
                sets.append(d_)
            Rt = [sb(es, "g_R%d" % i, [128, 128]) for i in range(2)]; BRt = [Buf("g_R%d" % i) for i in range(2)]
            VNt = [sb(es, "g_VN%d" % i, [128, 128]) for i in range(2)]; BVNt = [Buf("g_VN%d" % i) for i in range(2)]
            pbk = [ps(es, "g_pk%d" % i, [128, 512]) for i in range(6)]
            free_slots = [(pbk[i][:, 0:128], Buf("g_pk%d" % i)) for i in range(6)] + [(pbig[i][:, 0:128], Bpbig[i]) for i in range(2)]

            def slot():
                assert free_slots, "out of PSUM slots"
                return free_slots.pop(0)

            def rel(s_):
                free_slots.append(s_)

            def lockstep(gens):
                gens = list(gens)
                while gens:
                    for g_ in list(gens):
                        try:
                            next(g_)
                        except StopIteration:
                            gens.remove(g_)
            for i in range(3):
                kb.op("pool", lambda i=i: P_.memset(raw[i][:], 0.0), writes=[Braw[i]])

            def mm1(dst, Bd, lhsT, rhs, rds):
                kb.op("pe", lambda: PE.matmul(dst, lhsT, rhs, start=True, stop=True), reads=rds, writes=[Bd])

            def tr(dst, Bd, src, rds):
                kb.op("pe", lambda: PE.transpose(dst, src, IDENT), reads=rds + [Bcst], writes=[Bd])

            for h in range(8):
                for i in range(3):
                    ch = i * 8 + h
                    kb.dma("sp", raw[i][:, 2:258], qkvT[ch, :, 0:256], reads=[BqkvT], writes=[Braw[i]])
                    kb.dma("sp", raw[i][:, 262:2310], qkvT[ch, :, 256:T], reads=[BqkvT], writes=[Braw[i]], part=True)
                    for (d0, n, s0) in ((0, 256, 0), (256, 2048, 260)):
                        kb.op("dve", lambda i=i, ch=ch, d0=d0, n=n, s0=s0: V.tensor_scalar(cv_[i][:, d0:d0 + n], raw[i][:, s0:s0 + n], convT[:, ch:ch + 1], None, ALU.mult),
                              reads=[Braw[i], Bconv], writes=[Bcv[i]])
                        for tp in range(1, 5):
                            kb.op("dve", lambda i=i, ch=ch, d0=d0, n=n, s0=s0, tp=tp: V.scalar_tensor_tensor(
                                out=cv_[i][:, d0:d0 + n], in0=raw[i][:, s0 + tp:s0 + tp + n], scalar=convT[:, tp * 24 + ch:tp * 24 + ch + 1],
                                in1=cv_[i][:, d0:d0 + n], op0=ALU.mult, op1=ALU.add), reads=[Braw[i], Bconv, Bcv[i]], writes=[Bcv[i]])
                    kb.op("act", lambda i=i: S_.activation(cv_[i][:], cv_[i][:], AF.Silu), reads=[Bcv[i]], writes=[Bcv[i]])
                    if i < 2:
                        qs = DK_SCALE if i == 0 else 1.0
                        for ti, (n0, n1) in enumerate(NT5):
                            n = n1 - n0
                            pb_, Bpb_ = pbig[ti % 2], Bpbig[ti % 2]
                            kb.op("act", lambda i=i, n0=n0, n=n: S_.activation(tmp5[:, 0:n], cv_[i][:, n0:n0 + n], AF.Square), reads=[Bcv[i]], writes=[Btmp5])
                            kb.op("pe", lambda pb_=pb_, n=n: PE.matmul(pb_[:, 0:n], ONES, tmp5[:, 0:n], start=True, stop=True), reads=[Bcst, Btmp5], writes=[Bpb_])
                            kb.op("act", lambda pb_=pb_, n=n: S_.activation(tmp5[:, 0:n], pb_[:, 0:n], AF.Sqrt, bias=EPS, scale=1.0), reads=[Bpb_], writes=[Btmp5])
                            kb.op("dve", lambda n=n: V.reciprocal(tmp5[:, 0:n], tmp5[:, 0:n]), reads=[Btmp5], writes=[Btmp5])
                            kb.op("dve", lambda i=i, n0=n0, n=n, qs=qs: V.scalar_tensor_tensor(
                                out=cv_[i][:, n0:n0 + n], in0=cv_[i][:, n0:n0 + n], scalar=qs, in1=tmp5[:, 0:n], op0=ALU.mult, op1=ALU.mult),
                                reads=[Bcv[i], Btmp5], writes=[Bcv[i]])
                qT, kT, vT = cv_[0], cv_[1], cv_[2]
                BqT, BkT, BvT = Bcv
                for c in range(NTC):
                    sl, Bsl = slot()
                    tr(sl, Bsl, kT[:, c * 128:(c + 1) * 128], [BkT])
                    kb.op("act", lambda c=c, sl=sl: S_.copy(ktok[:, c, :], sl), reads=[Bsl], writes=[Bktok])
                    rel((sl, Bsl))
                    sl2, Bsl2 = slot()
                    tr(sl2, Bsl2, vT[:, c * 128:(c + 1) * 128], [BvT])
                    kb.op("dve", lambda c=c, sl2=sl2: V.tensor_copy(vtok[:, c, :], sl2), reads=[Bsl2], writes=[Bvtok])
                    rel((sl2, Bsl2))
                kb.op("pool", lambda: P_.memset(osum[:], 0.0), writes=[Bosum])

                def prep(c, d, st):
                    cs = slice(c * 128, (c + 1) * 128)
                    gcol = GC[:, d, c, h:h + 1]
                    MP = cst[:, K_MP0:K_MP0 + 128] if d == 0 else cst[:, K_MP1:K_MP1 + 128]
                    ST = cst[:, K_ST0:K_ST0 + 128] if d == 0 else cst[:, K_ST1:K_ST1 + 128]
                    DG, BDG = st["DG"]; TM, BTM = st["TM"]; DMS, BDMS = st["DMS"]
                    AN, BAN = st["AN"]; ATT, BATT = st["ATT"]; TT, BTT = st["TT"]
                    PA, BPA = st["PA"]; PB, BPB = st["PB"]; QA, BQA = st["QA"]; QB, BQB = st["QB"]
                    KDEC, BKDEC = st["KDEC"]; VB, BVB = st["VB"]
                    kb.op("dve", lambda: V.tensor_scalar(DG[:], IDENT, gcol, None, ALU.mult), reads=[Bcst, BGC], writes=[BDG])
                    kb.op("pool", lambda: P_.tensor_scalar(KDEC[:], ktok[:, c, :], KD[:, d, c, h:h + 1], None, ALU.mult), reads=[Bktok, BKD], writes=[BKDEC])
                    kb.op("pool", lambda: P_.tensor_scalar(VB[:], vtok[:, c, :], BE[:, d, c, h:h + 1], None, ALU.mult), reads=[Bvtok, BBE], writes=[BVB])
                    yield
                    s1, Bs1 = slot()
                    mm1(s1, Bs1, ONES, DG[:], [Bcst, BDG])
                    yield
                    kb.op("dve", lambda: V.scalar_tensor_tensor(out=TM[:], in0=s1, scalar=gcol, in1=MP, op0=ALU.subtract, op1=ALU.add),
                          reads=[Bs1, BGC, Bcst], writes=[BTM])
                    rel((s1, Bs1))
                    yield
                    kb.op("act", lambda: S_.activation(TM[:], TM[:], AF.Exp, scale=-1.0), reads=[BTM], writes=[BTM])
                    s2, Bs2 = slot()
                    mm1(s2, Bs2, kT[:, cs], kT[:, cs], [BkT])
                    yield
                    kb.op("pool", lambda: P_.tensor_tensor(DMS[:], TM[:], ST, ALU.mult), reads=[BTM, Bcst], writes=[BDMS])
                    yield
                    kb.op("dve", lambda: V.scalar_tensor_tensor(out=AN[:], in0=s2, scalar=NB[:, d, c, h:h + 1], in1=DMS[:], op0=ALU.mult, op1=ALU.mult),
                          reads=[Bs2, BNB, BDMS], writes=[BAN])
                    rel((s2, Bs2))
                    s3, Bs3 = slot()
                    mm1(s3, Bs3, qT[:, cs], kT[:, cs], [BqT, BkT])
                    yield
                    kb.op("dve", lambda: V.tensor_tensor(TM[:], s3, TM[:], ALU.mult), reads=[Bs3, BTM], writes=[BTM])
                    rel((s3, Bs3))
                    s5, Bs5 = slot()
                    tr(s5, Bs5, AN[:], [BAN])
                    yield
                    kb.op("dve", lambda: V.tensor_copy(QA[:], s5), reads=[Bs5], writes=[BQA])
                    kb.op("dve", lambda: V.tensor_tensor(TT[:], s5, IDENT, ALU.add), reads=[Bs5, Bcst], writes=[BTT])
                    rel((s5, Bs5))
                    s4, Bs4 = slot()
                    tr(s4, Bs4, TM[:], [BTM])
                    yield
                    kb.op("act", lambda: S_.copy(ATT[:], s4), reads=[Bs4], writes=[BATT])
                    rel((s4, Bs4))
                    P, BP, Q, BQ = AN, BAN, QA, BQA
                    Pn, BPn, Qn, BQn = PA, BPA, QB, BQB
                    for k in range(6):
                        sp_, Bsp = slot()
                        mm1(sp_, Bsp, Q[:], P[:], [BQ, BP])
                        yield
                        kb.op("act", lambda Pn=Pn, sp_=sp_: S_.copy(Pn[:], sp_), reads=[Bsp], writes=[BPn])
                        rel((sp_, Bsp))
                        if k < 5:
                            sq_, Bsq_ = slot()
                            mm1(sq_, Bsq_, P[:], Q[:], [BP, BQ])
                        yield
                        st_, Bst_ = slot()
                        mm1(st_, Bst_, Pn[:], TT[:], [BPn, BTT])
                        if k < 5:
                            kb.op("dve", lambda Qn=Qn, sq_=sq_: V.tensor_copy(Qn[:], sq_), reads=[Bsq_], writes=[BQn])
                            rel((sq_, Bsq_))
                        yield
                        kb.op("dve", lambda st_=st_: V.tensor_tensor(TT[:], TT[:], st_, ALU.add), reads=[BTT, Bst_], writes=[BTT])
                        rel((st_, Bst_))
                        yield
                        P, BP, Pn, BPn = Pn, BPn, (PB if Pn is PA else PA), (BPB if Pn is PA else BPA)
                        Q, BQ, Qn, BQn = Qn, BQn, (QA if Qn is QB else QB), (BQA if Qn is QB else BQB)

                def steps(d, cl, stl):
                    for c, st in zip(cl, stl):
                        cs = slice(c * 128, (c + 1) * 128)
                        S, BSd = Sst[d], BS[d]
                        R, BR = Rt[d], BRt[d]; VN, BVN = VNt[d], BVNt[d]
                        TT, BTT = st["TT"]; ATT, BATT = st["ATT"]
                        KDEC, BKDEC = st["KDEC"]; VB, BVB = st["VB"]
                        a1, Ba1 = slot()
                        mm1(a1, Ba1, kT[:, cs], S[:], [BkT, BSd])
                        a2, Ba2 = slot()
                        mm1(a2, Ba2, qT[:, cs], S[:], [BqT, BSd])
                        yield
                        kb.op("dve", lambda: V.scalar_tensor_tensor(out=R[:], in0=a1, scalar=NBEG[:, d, c, h:h + 1], in1=VB[:], op0=ALU.mult, op1=ALU.add),
                              reads=[Ba1, BNBEG, BVB], writes=[BR])
                        kb.op("dve", lambda: V.scalar_tensor_tensor(out=osum[:, c, :], in0=a2, scalar=EG[:, d, c, h:h + 1], in1=osum[:, c, :], op0=ALU.mult, op1=ALU.add),
                              reads=[Ba2, BEG_, Bosum], writes=[Bosum])
                        rel((a1, Ba1)); rel((a2, Ba2))
                        yield
                        a3, Ba3 = slot()
                        mm1(a3, Ba3, TT[:], R[:], [BTT, BR])
                        yield
                        kb.op("act", lambda: S_.copy(VN[:], a3), reads=[Ba3], writes=[BVN])
                        rel((a3, Ba3))
                        yield
                        a4, Ba4 = slot()
                        mm1(a4, Ba4, KDEC[:], VN[:], [BKDEC, BVN])
                        a5, Ba5 = slot()
                        mm1(a5, Ba5, ATT[:], VN[:], [BATT, BVN])
                        yield
                        kb.op("dve", lambda: V.scalar_tensor_tensor(out=S[:], in0=S[:], scalar=GL[:, d, c, h:h + 1], in1=a4, op0=ALU.mult, op1=ALU.add),
                              reads=[BSd, BGL, Ba4], writes=[BSd])
                        kb.op("dve", lambda: V.tensor_tensor(osum[:, c, :], osum[:, c, :], a5, ALU.add), reads=[Bosum, Ba5], writes=[Bosum])
                        rel((a4, Ba4)); rel((a5, Ba5))
                        yield

                orders = [list(range(NTC)), [1, 0] + list(range(NTC - 1, 1, -1))]
                for d in range(2):
                    kb.op("pool", lambda d=d: P_.memset(Sst[d][:], 0.0), writes=[BS[d]])
                for w0 in range(0, NTC, G_W):
                    gens = []
                    wave = []
                    for d in range(2):
                        cl = orders[d][w0:w0 + G_W]
                        stl = [sets[d * G_W + i] for i in range(len(cl))]
                        wave.append((d, cl, stl))
                        for c, st in zip(cl, stl):
                            gens.append(prep(c, d, st))
                    lockstep(gens)
                    lockstep([steps(d, cl, stl) for (d, cl, stl) in wave])

                kb.dma("sp", zt[:], z_tok[:, h * 128:(h + 1) * 128].rearrange("(c p) n -> p c n", p=128), reads=[Bz], writes=[Bzt])
                kb.op("act", lambda: S_.activation(zt[:], zt[:], AF.Silu), reads=[Bzt], writes=[Bzt])
                for c in range(NTC):
                    kb.op("act", lambda c=c: S_.activation(tmp5[:, 0:128], osum[:, c, :], AF.Square, accum_out=ssq[:, c:c + 1]), reads=[Bosum], writes=[Btmp5, Bssq])
                kb.op("act", lambda: S_.activation(ssq[:], ssq[:], AF.Sqrt, bias=EPS, scale=1.0 / 128), reads=[Bssq], writes=[Bssq])
                kb.op("dve", lambda: V.reciprocal(ssq[:], ssq[:]), reads=[Bssq], writes=[Bssq])
                kb.op("dve", lambda: V.tensor_tensor(osum[:], osum[:], ssq[:].unsqueeze(2).to_broadcast([128, 18, 128]), ALU.mult), reads=[Bosum, Bssq], writes=[Bosum])
                kb.op("dve", lambda: V.tensor_tensor(osum[:], osum[:], nwb[:].unsqueeze(1).to_broadcast([128, 18, 128]), ALU.mult), reads=[Bosum, Bnwb], writes=[Bosum])
                kb.op("dve", lambda: V.tensor_tensor(osum[:], osum[:], zt[:], ALU.mult), reads=[Bosum, Bzt], writes=[Bosum])
                for c in range(NTC):
                    sl, Bsl = slot()
                    tr(sl, Bsl, osum[:, c, :], [Bosum])
                    kb.op("act", lambda c=c, sl=sl: S_.copy(otb[:, c * 128:(c + 1) * 128], sl), reads=[Bsl], writes=[Botb])
                    rel((sl, Bsl))
                kb.dma("sp", OTd[h], otb[:], reads=[Botb], writes=[BOT])
            phase_end()

    Zs = dscr("na_Zs", [120, 64, 128]); BZs = Buf("na_Zs")

    def phase_attn(l, ctx_out):
        c_first = 0 if ctx_out else 2
        with ExitStack() as es:
            qB = sb(es, "a_qB", [64, 8, T], BF16); BqB = Buf("a_qB")
            kB = sb(es, "a_kB", [64, 2, T], BF16); BkB = Buf("a_kB")
            vaB = sb(es, "a_vaB", [128, 18, 2, 65], BF16); BvaB = Buf("a_vaB")
            sinke = sb(es, "a_sink", [128, 8]); Bsink = Buf("a_sink")
            otok = sb(es, "a_otok", [128, 512]); Botok = Buf("a_otok")
            otokb = sb(es, "a_otokb", [128, 512], BF16); Botokb = Buf("a_otokb")
            OTs = sb(es, "a_OTs", [128, 4, T], BF16); BOTs = Buf("a_OTs")
            den = sb(es, "a_den", [128, 8]); Bden = Buf("a_den")
            tmpS = [sb(es, "a_tmpS%d" % i, [128, 512]) for i in range(2)]; BtmpS = [Buf("a_tmpS%d" % i) for i in range(2)]
            PT = [sb(es, "a_PT%d" % i, [128, 512], BF16) for i in range(3)]; BPT = [Buf("a_PT%d" % i) for i in range(3)]
            pS = [ps(es, "a_pS%d" % i, [128, 512]) for i in range(3)]; BpS = [Buf("a_pS%d" % i) for i in range(3)]
            pO = [ps(es, "a_pO%d" % i, [128, 512]) for i in range(2)]; BpO = [Buf("a_pO%d" % i) for i in range(2)]
            pT = [ps(es, "a_pT%d" % i, [128, 512], BF16) for i in range(2)]; BpT = [Buf("a_pT%d" % i) for i in range(2)]
            kb.dma("sp", qB[:], qBT.rearrange("h d t -> d h t"), reads=[BqBT], writes=[BqB])
            kb.dma("sp", kB[:], kBT.rearrange("h d t -> d h t"), reads=[BkBT], writes=[BkB])
            kb.op("pool", lambda: P_.memset(vaB[:], 1.0), writes=[BvaB])
            for g_ in range(2):
                kb.dma("sp", vaB[:, :, g_, 0:64], vB[:, g_ * 64:(g_ + 1) * 64].rearrange("(c p) d -> p c d", p=128), reads=[BvB], writes=[BvaB], part=(g_ > 0))
            kb.dma("sp", sinke[:], bcast_rows(sink_b[l, 0]), reads=[Bin], writes=[Bsink])
            kb.op("act", lambda: S_.activation(sinke[:], sinke[:], AF.Exp), reads=[Bsink], writes=[Bsink])
            cnt = {"s": 0, "o": 0, "p": 0, "t": 0}
            WPREV = cst[:, K_WPREV:K_WPREV + 128]
            WNEXT = cst[:, K_WNEXT:K_WNEXT + 128]

            def finish_chunk(c, nheads_done_buf=None):
                kb.op("act", lambda: S_.copy(otokb[:], otok[:]), reads=[Botok], writes=[Botokb])
                ti = cnt["t"] % 2; cnt["t"] += 1
                for i in range(4):
                    kb.op("pe", lambda i=i: PE.transpose(pT[ti][:, i * 128:(i + 1) * 128], otokb[:, i * 128:(i + 1) * 128], identb[:]),
                          reads=[Botokb, Bidb], writes=[BpT[ti]])
                kb.op("dve", lambda: V.tensor_copy(OTs[:, :, c * 128:(c + 1) * 128], pT[ti][:].rearrange("p (i t) -> p i t", i=4)),
                      reads=[BpT[ti]], writes=[BOTs])

            for c in range(c_first, NTC):
                for gk in range(2):
                    if c < 2:
                        keys = [(0, None), (1, None)]
                    else:
                        keys = []
                        if c > 2:
                            keys.append((c - 1, WPREV))
                        keys.append((c, None))
                        if c < NTC - 1:
                            keys.append((c + 1, WNEXT))
                        keys += [(0, None), (1, None)]
                    oi = cnt["o"] % 2; cnt["o"] += 1
                    rhs = qB[:, 4 * gk:4 * gk + 4, c * 128:(c + 1) * 128]
                    for ki, (kc, mask) in enumerate(keys):
                        si = cnt["s"] % 3; cnt["s"] += 1
                        kb.op("pe", lambda si=si, kc=kc: PE.matmul(pS[si][:].rearrange("p (h t) -> p h t", h=4), kB[:, gk, kc * 128:(kc + 1) * 128], rhs, start=True, stop=True),
                              reads=[BkB, BqB], writes=[BpS[si]])
                        pi = cnt["p"] % 3; cnt["p"] += 1
                        if mask is not None:
                            mi = cnt["s"] % 2
                            kb.op("dve", lambda si=si, mi=mi, mask=mask: V.tensor_tensor(tmpS[mi][:].rearrange("p (h t) -> p h t", h=4), pS[si][:].rearrange("p (h t) -> p h t", h=4),
                                                                                     mask.unsqueeze(1).to_broadcast([128, 4, 128]), ALU.add),
                                  reads=[BpS[si], Bcst], writes=[BtmpS[mi]])
                            kb.op("act", lambda mi=mi, pi=pi: S_.activation(PT[pi][:], tmpS[mi][:], AF.Exp), reads=[BtmpS[mi]], writes=[BPT[pi]])
                        else:
                            kb.op("act", lambda si=si, pi=pi: S_.activation(PT[pi][:], pS[si][:], AF.Exp), reads=[BpS[si]], writes=[BPT[pi]])
                        for hh in range(4):
                            first = (ki == 0 and hh == 0)
                            last = (ki == len(keys) - 1 and hh == 3)
                            kb.op("pe", lambda hh=hh, pi=pi, kc=kc, first=first, last=last: PE.matmul(
                                pO[oi][:, hh * 65:(hh + 1) * 65], PT[pi][:, hh * 128:(hh + 1) * 128], vaB[:, kc, gk, :],
                                start=first, stop=last, skip_group_check=True), reads=[BPT[pi], BvaB], writes=[BpO[oi]])
                    pov = pO[oi][:, 0:260].rearrange("p (h e) -> p h e", e=65)
                    kb.op("dve", lambda pov=pov: V.tensor_tensor(den[:, 0:4], pov[:, :, 64], sinke[:, 4 * gk:4 * gk + 4], ALU.add), reads=[BpO[oi], Bsink], writes=[Bden])
                    kb.op("dve", lambda: V.reciprocal(den[:, 0:4], den[:, 0:4]), reads=[Bden], writes=[Bden])
                    kb.op("dve", lambda pov=pov: V.tensor_tensor(otok[:, gk * 256:(gk + 1) * 256].rearrange("p (h e) -> p h e", e=64), pov[:, :, 0:64],
                                                                 den[:, 0:4].unsqueeze(2).to_broadcast([128, 4, 64]), ALU.mult), reads=[BpO[oi], Bden], writes=[Botok])
                finish_chunk(c)
            for i in range(4):
                kb.dma("sp", OTd[8 + i], OTs[:, i, :], reads=[BOTs], writes=[BOT])
            phase_end()

        with ExitStack() as es:
            qC = sb(es, "c_qC", [64, 8, T], BF16); BqC = Buf("c_qC")
            kC = sb(es, "c_kC", [64, 8, T], BF16); BkC = Buf("c_kC")
            vaC = sb(es, "c_vaC", [128, 18, 8, 65], BF16); BvaC = Buf("c_vaC")
            BTb = sb(es, "c_BT", [128, 120, 64]); BBTb = Buf("c_BT")
            otok = sb(es, "c_otok", [128, 512]); Botok = Buf("c_otok")
            otokb = sb(es, "c_otokb", [128, 512], BF16); Botokb = Buf("c_otokb")
            OTs = sb(es, "c_OTs", [128, 4, T], BF16); BOTs = Buf("c_OTs")
            den = sb(es, "c_den", [128, 8]); Bden = Buf("c_den")
            tmpS = [sb(es, "c_tmpS%d" % i, [128, 128]) for i in range(3)]; BtmpS = [Buf("c_tmpS%d" % i) for i in range(3)]
            PT = [sb(es, "c_PT%d" % i, [128, 128], BF16) for i in range(3)]; BPT = [Buf("c_PT%d" % i) for i in range(3)]
            pS = [ps(es, "c_pS%d" % i, [128, 512]) for i in range(3)]; BpS = [Buf("c_pS%d" % i) for i in range(3)]
            pO = [ps(es, "c_pO%d" % i, [128, 512]) for i in range(2)]; BpO = [Buf("c_pO%d" % i) for i in range(2)]
            pT = [ps(es, "c_pT%d" % i, [128, 512], BF16) for i in range(2)]; BpT = [Buf("c_pT%d" % i) for i in range(2)]
            kb.dma("sp", qC[:], qCT.rearrange("h d t -> d h t"), reads=[BqCT], writes=[BqC])
            kb.dma("sp", kC[:], kCT.rearrange("h d t -> d h t"), reads=[BkCT], writes=[BkC])
            kb.op("pool", lambda: P_.memset(vaC[:], 1.0), writes=[BvaC])
            for g_ in range(8):
                kb.dma("sp", vaC[:, :, g_, 0:64], vC[:, g_ * 64:(g_ + 1) * 64].rearrange("(c p) d -> p c d", p=128), reads=[BvC], writes=[BvaC], part=(g_ > 0))
            rp = rpb_pad[l]
            kb.dma("sp", Zs, bass.AP(rp.tensor, rp.offset, [[128, 120], [0, 64], [1, 128]]), reads=[Bin], writes=[BZs], sem_buf=BZs)
            skew = bass.AP(Zs.tensor, Zs.offset + 63, [[127, 64], [64 * 128, 120], [1, 64]])
            kb.dma("sp", BTb[0:64], skew, reads=[BZs], writes=[BBTb])
            kb.dma("sp", BTb[64:128], skew, reads=[BZs], writes=[BBTb], part=True)
            kb.op("dve", lambda: V.tensor_tensor(BTb[:], BTb[:], cst[:, K_CM:K_CM + 64].unsqueeze(1).to_broadcast([128, 120, 64]), ALU.add),
                  reads=[BBTb, Bcst], writes=[BBTb])
            cnt = {"s": 0, "o": 0, "p": 0, "t": 0, "m": 0}

            def finish_chunk(c):
                kb.op("act", lambda: S_.copy(otokb[:], otok[:]), reads=[Botok], writes=[Botokb])
                ti = cnt["t"] % 2; cnt["t"] += 1
                for i in range(4):
                    kb.op("pe", lambda i=i: PE.transpose(pT[ti][:, i * 128:(i + 1) * 128], otokb[:, i * 128:(i + 1) * 128], identb[:]),
                          reads=[Botokb, Bidb], writes=[BpT[ti]])
                kb.op("dve", lambda: V.tensor_copy(OTs[:, :, c * 128:(c + 1) * 128], pT[ti][:].rearrange("p (i t) -> p i t", i=4)),
                      reads=[BpT[ti]], writes=[BOTs])

            def win(r):
                r0 = min(max(r - 4, 0), 24)
                return r0, r0 + 8

            for c in range(c_first, NTC):
                for h in range(8):
                    keys = []
                    if c >= 2:
                        qc = c - 2
                        rows = (2 * qc, 2 * qc + 1)
                        lo = min(win(r)[0] for r in rows); hi = max(win(r)[1] for r in rows)
                        for kcl in range(lo // 2, (hi + 1) // 2):
                            subs = []
                            for krl in range(2):
                                for qrl in range(2):
                                    kr = 2 * kcl + krl; r = rows[qrl]
                                    w0, w1 = win(r)
                                    subs.append((krl, qrl, (kr - r) if (w0 <= kr < w1) else None))
                            if any(s_[2] is not None for s_ in subs):
                                keys.append((kcl + 2, subs))
                    keys += [(0, None), (1, None)]
                    oi = cnt["o"] % 2; cnt["o"] += 1
                    rhs = qC[:, h, c * 128:(c + 1) * 128]
                    for ki, (kc, subs) in enumerate(keys):
                        si = cnt["s"] % 3; cnt["s"] += 1
                        kb.op("pe", lambda si=si, kc=kc: PE.matmul(pS[si][:, 0:128], kC[:, h, kc * 128:(kc + 1) * 128], rhs, start=True, stop=True),
                              reads=[BkC, BqC], writes=[BpS[si]])
                        pi = cnt["p"] % 3; cnt["p"] += 1
                        if subs is not None:
                            mi = cnt["m"] % 3; cnt["m"] += 1
                            for (krl, qrl, dr) in subs:
                                ps_ = slice(krl * 64, (krl + 1) * 64); fs_ = slice(qrl * 64, (qrl + 1) * 64)
                                if dr is None:
                                    kb.op("pool", lambda mi=mi, ps_=ps_, fs_=fs_: P_.memset(tmpS[mi][ps_, fs_], -30000.0), writes=[BtmpS[mi]])
                                else:
                                    kb.op("dve", lambda mi=mi, si=si, ps_=ps_, fs_=fs_, dr=dr: V.tensor_tensor(
                                        tmpS[mi][ps_, fs_], pS[si][ps_, fs_], BTb[ps_, h * 15 + dr + 7, :], ALU.add),
                                        reads=[BpS[si], BBTb], writes=[BtmpS[mi]])
                            kb.op("act", lambda mi=mi, pi=pi: S_.activation(PT[pi][:], tmpS[mi][:], AF.Exp), reads=[BtmpS[mi]], writes=[BPT[pi]])
                        else:
                            kb.op("act", lambda si=si, pi=pi: S_.activation(PT[pi][:], pS[si][:, 0:128], AF.Exp), reads=[BpS[si]], writes=[BPT[pi]])
                        kb.op("pe", lambda pi=pi, kc=kc, ki=ki, nk=len(keys): PE.matmul(pO[oi][:, 0:65], PT[pi][:], vaC[:, kc, h, :],
                                                                                   start=(ki == 0), stop=(ki == nk - 1)), reads=[BPT[pi], BvaC], writes=[BpO[oi]])
                    kb.op("dve", lambda: V.reciprocal(den[:, 0:1], pO[oi][:, 64:65]), reads=[BpO[oi]], writes=[Bden])
                    kb.op("dve", lambda: V.tensor_scalar(otok[:, h * 64:(h + 1) * 64], pO[oi][:, 0:64], den[:, 0:1], None, ALU.mult),
                          reads=[BpO[oi], Bden], writes=[Botok])
                finish_chunk(c)
            for i in range(4):
                kb.dma("sp", OTd[12 + i], OTs[:, i, :], reads=[BOTs], writes=[BOT])
            phase_end()

    def make_gateB(es, w):
        gB = sb(es, "gB", [128, 2, D]); BgB = Buf("gB")
        g0 = 32 if w == 0 else 80
        with ExitStack() as e2:
            dg = sb(e2, "dg", [128, 128]); Bdg = Buf("dg")
            pg = ps(e2, "pg", [128, 512]); Bpg = Buf("pg")
            for j in range(2):
                for c4 in range(4):
                    for cc in range(4):
                        c = c4 * 4 + cc
                        kb.op("dve", lambda c=c, j=j: V.tensor_scalar(dg[:], IDENT, modT[:, g0 + c, j:j + 1], None, ALU.mult),
                              reads=[Bcst, Bmod, Bpg], writes=[Bdg])
                        kb.op("pe", lambda cc=cc: PE.matmul(pg[:, cc * 128:(cc + 1) * 128], ONES, dg[:], start=True, stop=True),
                              reads=[Bcst, Bdg], writes=[Bpg])
                    kb.op("act", lambda j=j, c4=c4: S_.copy(gB[:, j, c4 * 512:(c4 + 1) * 512], pg[:]), reads=[Bpg], writes=[BgB])
            kb.barrier()
        return gB, BgB

    def phase_out(l, which_src, which_dst, c_first):
        with ExitStack() as es:
            gB, BgB = make_gateB(es, 0)
            OT = sb(es, "o_OT", [128, 16, T], BF16); BOTs_ = Buf("o_OT")
            wo = [sb(es, "o_wo%d" % i, [128, 16, 512], BF16) for i in range(2)]; Bwo = [Buf("o_wo%d" % i) for i in range(2)]
            xc = [sb(es, "o_xc%d" % i, [128, 512]) for i in range(3)]; Bxc = [Buf("o_xc%d" % i) for i in range(3)]
            pp = [ps(es, "o_pp%d" % i, [128, 512]) for i in range(3)]; Bpp = [Buf("o_pp%d" % i) for i in range(3)]
            kb.dma("sp", OT[:], OTd.rearrange("c p t -> p c t"), reads=[BOT], writes=[BOTs_])
            w_v = w_out[l].rearrange("(cc p) n -> p cc n", p=128)
            k = 0
            for dt in range(4):
                kb.dma("pool", wo[dt % 2][:], w_v[:, :, dt * 512:(dt + 1) * 512], reads=[Bin], writes=[Bwo[dt % 2]])
                for c in range(c_first, NTC):
                    j = 0 if c >= 2 else 1
                    src, Bsrc = x_src(which_src, c)
                    i = k % 3; k += 1
                    kb.dma("sp", xc[i][:], src[:, dt * 512:(dt + 1) * 512], reads=[Bsrc], writes=[Bxc[i]])
                    for cc in range(16):
                        kb.op("pe", lambda cc=cc: PE.matmul(pp[i][:], OT[:, cc, c * 128:(c + 1) * 128], wo[dt % 2][:, cc, :], start=(cc == 0), stop=(cc == 15)),
                              reads=[BOTs_, Bwo[dt % 2]], writes=[Bpp[i]])
                    tmpo = sb_tmp_o[0]
                    kb.op("dve", lambda: V.tensor_tensor(tmpo[:], pp[i][:], gB[:, j, dt * 512:(dt + 1) * 512], ALU.mult), reads=[Bpp[i], BgB], writes=[Btmp_o])
                    kb.op("dve", lambda: V.tensor_tensor(xc[i][:], xc[i][:], tmpo[:], ALU.add), reads=[Bxc[i], Btmp_o], writes=[Bxc[i]])
                    kb.dma("sp", xs[which_dst][c * 128:(c + 1) * 128, dt * 512:(dt + 1) * 512], xc[i][:], reads=[Bxc[i]], writes=[Bxs[which_dst]])
            phase_end()

    sb_tmp_o = [sb(es_all, "tmp_o", [128, 512])]; Btmp_o = Buf("tmp_o")
    fTd = dscr("fTd", [16, 128, T], BF16); BfTd = Buf("fTd")

    def phase_ffn(l, which_src, which_dst, c_first):
        Wt = sb(es_all, "Wt%d" % l, [128, 18, 32]); BWt = Buf("Wt")
        with ExitStack() as es:
            fT = sb(es, "f_fT", [128, 16, T], BF16); BfT = Buf("f_fT")
            wr_sb = sb(es, "f_wr", [128, 16, 36]); Bwr = Buf("f_wr")
            rl = sb(es, "f_rl", [128, 18, 36]); Brl = Buf("f_rl")
            brb = sb(es, "f_brb", [128, 36]); Bbrb = Buf("f_brb")
            kb.dma("sp", wr_sb[:], w_r[l].rearrange("(dc p) n -> p dc n", p=128), reads=[Bin], writes=[Bwr])
            kb.dma("sp", brb[:], bcast_rows(b_r[l, 0]), reads=[Bin], writes=[Bbrb])
            phase_norm(l, 1, which_src, fT, BfT, router=(wr_sb, Bwr, rl, Brl), c_lo=c_first)
            kb.dma("sp", fTd.rearrange("c p t -> p c t"), fT[:], reads=[BfT], writes=[BfTd])
            t4 = sb(es, "f_t4", [128, 18, 4]); Bt4 = Buf("f_t4")
            gm = sb(es, "f_gm", [128, 18, 4]); Bgm = Buf("f_gm")
            s1 = sb(es, "f_s1", [128, 18]); Bs1 = Buf("f_s1")
            gp = sb(es, "f_gp", [128, 18]); Bgp = Buf("f_gp")
            el = sb(es, "f_el", [128, 18, 32]); Bel = Buf("f_el")
            m1 = sb(es, "f_m1", [128, 18]); Bm1 = Buf("f_m1")
            m2 = sb(es, "f_m2", [128, 18]); Bm2 = Buf("f_m2")
            k1 = sb(es, "f_k1", [128, 18, 32]); Bk1 = Buf("f_k1")
            k2 = sb(es, "f_k2", [128, 18, 32]); Bk2 = Buf("f_k2")
            p1 = sb(es, "f_p1", [128, 18]); Bp1 = Buf("f_p1")
            p2 = sb(es, "f_p2", [128, 18]); Bp2 = Buf("f_p2")
            bc32 = lambda a: a[:].unsqueeze(2).to_broadcast([128, 18, 32])
            bc4 = lambda a: a[:].unsqueeze(2).to_broadcast([128, 18, 4])
            kb.op("dve", lambda: V.tensor_tensor(rl[:], rl[:], brb[:].unsqueeze(1).to_broadcast([128, 18, 36]), ALU.add), reads=[Brl, Bbrb], writes=[Brl])
            kb.op("dve", lambda: V.tensor_reduce(s1[:], rl[:, :, 0:4], AX.X, ALU.max), reads=[Brl], writes=[Bs1])
            kb.op("dve", lambda: V.tensor_tensor(gm[:], rl[:, :, 0:4], bc4(s1), ALU.is_equal), reads=[Brl, Bs1], writes=[Bgm])
            kb.op("dve", lambda: V.tensor_tensor(t4[:], rl[:, :, 0:4], bc4(s1), ALU.subtract), reads=[Brl, Bs1], writes=[Bt4])
            kb.op("act", lambda: S_.activation(t4[:], t4[:], AF.Exp), reads=[Bt4], writes=[Bt4])
            kb.op("dve", lambda: V.tensor_reduce(gp[:], t4[:], AX.X, ALU.add), reads=[Bt4], writes=[Bgp])
            kb.op("dve", lambda: V.reciprocal(gp[:], gp[:]), reads=[Bgp], writes=[Bgp])
            kb.op("dve", lambda: V.tensor_scalar(gm[:], gm[:], -1.0, 10000.0, ALU.add, ALU.mult), reads=[Bgm], writes=[Bgm])
            kb.op("dve", lambda: V.tensor_tensor(el[:].rearrange("p c (g e) -> p c g e", e=8), rl[:, :, 4:36].rearrange("p c (g e) -> p c g e", e=8),
                                                 gm[:].unsqueeze(3).to_broadcast([128, 18, 4, 8]), ALU.add), reads=[Brl, Bgm], writes=[Bel])
            kb.op("dve", lambda: V.tensor_reduce(m1[:], el[:], AX.X, ALU.max), reads=[Bel], writes=[Bm1])
            kb.op("dve", lambda: V.tensor_tensor(k1[:], el[:], bc32(m1), ALU.is_equal), reads=[Bel, Bm1], writes=[Bk1])
            kb.op("dve", lambda: V.scalar_tensor_tensor(out=el[:], in0=k1[:], scalar=-10000.0, in1=el[:], op0=ALU.mult, op1=ALU.add), reads=[Bk1, Bel], writes=[Bel])
            kb.op("dve", lambda: V.tensor_reduce(m2[:], el[:], AX.X, ALU.max), reads=[Bel], writes=[Bm2])
            kb.op("dve", lambda: V.tensor_tensor(k2[:], el[:], bc32(m2), ALU.is_equal), reads=[Bel, Bm2], writes=[Bk2])
            kb.op("dve", lambda: V.tensor_tensor(p2[:], m2[:], m1[:], ALU.subtract), reads=[Bm1, Bm2], writes=[Bp2])
            kb.op("act", lambda: S_.activation(p2[:], p2[:], AF.Exp), reads=[Bp2], writes=[Bp2])
            kb.op("dve", lambda: V.tensor_scalar(p1[:], p2[:], 1.0, None, ALU.add), reads=[Bp2], writes=[Bp1])
            kb.op("dve", lambda: V.reciprocal(p1[:], p1[:]), reads=[Bp1], writes=[Bp1])
            kb.op("dve", lambda: V.tensor_tensor(p2[:], p2[:], p1[:], ALU.mult), reads=[Bp2, Bp1], writes=[Bp2])
            kb.op("dve", lambda: V.tensor_tensor(p1[:], p1[:], gp[:], ALU.mult), reads=[Bp1, Bgp], writes=[Bp1])
            kb.op("dve", lambda: V.tensor_tensor(p2[:], p2[:], gp[:], ALU.mult), reads=[Bp2, Bgp], writes=[Bp2])
            kb.op("dve", lambda: V.tensor_tensor(k1[:], k1[:], bc32(p1), ALU.mult), reads=[Bk1, Bp1], writes=[Bk1])
            kb.op("dve", lambda: V.tensor_tensor(k2[:], k2[:], bc32(p2), ALU.mult), reads=[Bk2, Bp2], writes=[Bk2])
            kb.op("dve", lambda: V.tensor_tensor(Wt[:], k1[:], k2[:], ALU.add), reads=[Bk1, Bk2], writes=[BWt])
            if debug:
                dW = nc.dram_tensor("dbg_W%d" % l, [128, 18 * 32], F32, kind="ExternalOutput").ap()
                kb.dma("sp", dW, Wt[:].rearrange("p c e -> p (c e)"), reads=[BWt], sem_buf=BWt)
            phase_end()
        if stop == "router":
            return
        chunks = list(range(c_first, NTC))
        ng = 3
        per = (len(chunks) + ng - 1) // ng
        groups = [chunks[i * per:(i + 1) * per] for i in range(ng)]
        with ExitStack() as es:
            gB, BgB = make_gateB(es, 1)
            f3 = sb(es, "m_f3", [128, 16, 768], BF16); Bf3 = Buf("m_f3")
            acc = sb(es, "m_acc", [128, 6, D]); Bacc = Buf("m_acc")
            HT = sb(es, "m_HT", [128, FC, 768], BF16); BHT = Buf("m_HT")
            wg = [sb(es, "m_wg%d" % i, [128, 16, 128], BF16) for i in range(2)]; Bwg = [Buf("m_wg%d" % i) for i in range(2)]
            wu = [sb(es, "m_wu%d" % i, [128, 16, 128], BF16) for i in range(2)]; Bwu = [Buf("m_wu%d" % i) for i in range(2)]
            wd = [sb(es, "m_wd%d" % i, [128, FC, 512], BF16) for i in range(2)]; Bwd = [Buf("m_wd%d" % i) for i in range(2)]
            sg = [sb(es, "m_sg%d" % i, [128, 512]) for i in range(2)]; Bsg = [Buf("m_sg%d" % i) for i in range(2)]
            xc = [sb(es, "m_xc%d" % i, [128, D]) for i in range(2)]; Bxc = [Buf("m_xc%d" % i) for i in range(2)]
            pG = [ps(es, "m_pG%d" % i, [128, 512]) for i in range(2)]; BpG = [Buf("m_pG%d" % i) for i in range(2)]
            pU = [ps(es, "m_pU%d" % i, [128, 512]) for i in range(2)]; BpU = [Buf("m_pU%d" % i) for i in range(2)]
            pY = [ps(es, "m_pY%d" % i, [128, 512]) for i in range(3)]; BpY = [Buf("m_pY%d" % i) for i in range(3)]
            kq = {"w": 0, "d": 0, "g": 0, "y": 0, "s": 0, "x": 0}
            for grp in groups:
                ntok = len(grp) * 128
                t0 = grp[0] * 128
                kb.dma("sp", f3[:, :, 0:ntok], fTd[:, :, t0:t0 + ntok].rearrange("c p t -> p c t"), reads=[BfTd], writes=[Bf3])
                kb.op("pool", lambda: P_.memset(acc[:], 0.0), writes=[Bacc])
                ntiles = [(0, min(512, ntok))] + ([(512, ntok)] if ntok > 512 else [])
                for e in range(ne_run):
                    wgv = w_gate[l, e].rearrange("(dc p) f -> p dc f", p=128)
                    wuv = w_up[l, e].rearrange("(dc p) f -> p dc f", p=128)
                    wdv = w_down[l, e].rearrange("(fc p) n -> p fc n", p=128)
                    for fc in range(FC):
                        wi = kq["w"] % 2; kq["w"] += 1
                        kb.dma("pool", wg[wi][:], wgv[:, :, fc * 128:(fc + 1) * 128], reads=[Bin], writes=[Bwg[wi]])
                        kb.dma("pool", wu[wi][:], wuv[:, :, fc * 128:(fc + 1) * 128], reads=[Bin], writes=[Bwu[wi]])
                        for (n0, n1) in ntiles:
                            gi = kq["g"] % 2; kq["g"] += 1
                            n = n1 - n0
                            for dc in range(DC):
                                kb.op("pe", lambda dc=dc: PE.matmul(pG[gi][:, 0:n], wg[wi][:, dc, :], f3[:, dc, n0:n1], start=(dc == 0), stop=(dc == DC - 1)),
                                      reads=[Bwg[wi], Bf3], writes=[BpG[gi]])
                            for dc in range(DC):
                                kb.op("pe", lambda dc=dc: PE.matmul(pU[gi][:, 0:n], wu[wi][:, dc, :], f3[:, dc, n0:n1], start=(dc == 0), stop=(dc == DC - 1)),
                                      reads=[Bwu[wi], Bf3], writes=[BpU[gi]])
                            si = kq["s"] % 2; kq["s"] += 1
                            kb.op("act", lambda: S_.activation(sg[si][:, 0:n], pG[gi][:, 0:n], AF.Silu), reads=[BpG[gi]], writes=[Bsg[si]])
                            kb.op("dve", lambda: V.tensor_tensor(HT[:, fc, n0:n1], sg[si][:, 0:n], pU[gi][:, 0:n], ALU.mult), reads=[Bsg[si], BpU[gi]], writes=[BHT])
                    for dt in range(4):
                        di = kq["d"] % 2; kq["d"] += 1
                        kb.dma("pool", wd[di][:], wdv[:, :, dt * 512:(dt + 1) * 512], reads=[Bin], writes=[Bwd[di]])
                        for ti, c in enumerate(grp):
                            yi = kq["y"] % 3; kq["y"] += 1
                            for fc in range(FC):
                                kb.op("pe", lambda fc=fc: PE.matmul(pY[yi][:], HT[:, fc, ti * 128:(ti + 1) * 128], wd[di][:, fc, :], start=(fc == 0), stop=(fc == FC - 1)),
                                      reads=[BHT, Bwd[di]], writes=[BpY[yi]])
                            kb.op("dve", lambda: V.scalar_tensor_tensor(out=acc[:, ti, dt * 512:(dt + 1) * 512], in0=pY[yi][:], scalar=Wt[:, c, e:e + 1],
                                                                        in1=acc[:, ti, dt * 512:(dt + 1) * 512], op0=ALU.mult, op1=ALU.add),
                                  reads=[BpY[yi], BWt, Bacc], writes=[Bacc])
                for ti, c in enumerate(grp):
                    j = 0 if c >= 2 else 1
                    src, Bsrc = x_src(which_src, c)
                    xi = kq["x"] % 2; kq["x"] += 1
                    kb.dma("sp", xc[xi][:], src, reads=[Bsrc], writes=[Bxc[xi]])
                    kb.op("dve", lambda: V.tensor_tensor(acc[:, ti, :], acc[:, ti, :], gB[:, j, :], ALU.mult), reads=[Bacc, BgB], writes=[Bacc])
                    kb.op("dve", lambda: V.tensor_tensor(xc[xi][:], xc[xi][:], acc[:, ti, :], ALU.add), reads=[Bxc[xi], Bacc], writes=[Bxc[xi]])
                    kb.dma("sp", xs[which_dst][c * 128:(c + 1) * 128, :], xc[xi][:], reads=[Bxc[xi]], writes=[Bxs[which_dst]])
            phase_end()

    def phase_final(which_src):
        with ExitStack() as es:
            nfB = sb(es, "z_nfB", [128, D]); BnfB = Buf("z_nfB")
            xt = [sb(es, "z_x%d" % i, [128, D]) for i in range(2)]; Bxt = [Buf("z_x%d" % i) for i in range(2)]
            sq = sb(es, "z_sq", [128, D]); Bsq = Buf("z_sq")
            ss = sb(es, "z_ss", [128, 2]); Bss = Buf("z_ss")
            kb.dma("sp", nfB[:], bcast_rows(norm_final[0]), reads=[Bin], writes=[BnfB])
            for c in range(2, NTC):
                i = c % 2
                src, Bsrc = x_src(which_src, c)
                kb.dma("sp", xt[i][:], src, reads=[Bsrc], writes=[Bxt[i]])
                kb.op("act", lambda: S_.activation(sq[:], xt[i][:], AF.Square, accum_out=ss[:, 0:1]), reads=[Bxt[i]], writes=[Bsq, Bss])
                kb.op("act", lambda: S_.activation(ss[:, 1:2], ss[:, 0:1], AF.Sqrt, bias=EPS, scale=1.0 / D), reads=[Bss], writes=[Bss])
                kb.op("dve", lambda: V.reciprocal(ss[:, 1:2], ss[:, 1:2]), reads=[Bss], writes=[Bss])
                kb.op("dve", lambda: V.scalar_tensor_tensor(out=xt[i][:], in0=xt[i][:], scalar=ss[:, 1:2], in1=nfB[:], op0=ALU.mult, op1=ALU.mult),
                      reads=[Bxt[i], Bss, BnfB], writes=[Bxt[i]])
                kb.dma("sp", out[(c - 2) * 128:(c - 1) * 128, :], xt[i][:], reads=[Bxt[i]], writes=[Bout])
            phase_end()
    Bout = Buf("out")

    def run_all():
        for l in range(depth):
            last = (l == 1)
            c_first = 2 if last else 0
            phase_mod(l)
            if stop == "mod":
                return
            with ExitStack() as es_h:
                hT = sb(es_h, "hT", [128, 16, T], BF16); BhT = Buf("hT")
                phase_norm(l, 0, 2 * l - 1, hT, BhT)
                if stop == "norm":
                    return
                phase_proj(l, hT, BhT)
            if stop == "proj":
                return
            if stop != "attn_only":
                phase_gdn(l)
            if stop == "gdn":
                return
            phase_attn(l, not last)
            if stop in ("attn", "attn_only"):
                return
            phase_out(l, 2 * l - 1, 2 * l, c_first)
            if stop == "out":
                return
            phase_ffn(l, 2 * l, 2 * l + 1, c_first)
            if stop in ("router", "moe"):
                return
        phase_final(2 * depth - 1)
    g.__dict__.update(locals())
    run_all()
    if debug:
        for nm_, t_, B_ in (("modT", modT, Bmod), ("modA", modA, BmodA)):
            d_ = nc.dram_tensor("dbg_" + nm_, [128, int(np.prod(t_.shape[1:]))], F32, kind="ExternalOutput").ap()
            kb.dma("sp", d_, t_[:].rearrange("p ... -> p (...)") if len(t_.shape) > 2 else t_[:], reads=[B_], sem_buf=B_)
    kb.finish()
    es_all.close()
    print("build: inst", kb.n_inst, "waits", kb.n_wait, "dsems", kb.ndsem)
    return g


def make_in_maps(inp, need_moe=True, ncores=8):
    f = lambda a: np.ascontiguousarray(np.asarray(a, dtype=np.float32))
    shared = {}
    shared["w_mod"] = f(inp["w_mod"])
    shared["b_mod"] = f(inp["b_mod"]).reshape(2, 96, 128)
    shared["norm_mix"] = f(inp["norm_mix"]).reshape(2, 16, 128)
    shared["norm_ffn"] = f(inp["norm_ffn"]).reshape(2, 16, 128)
    shared["norm_final"] = f(inp["norm_final"]).reshape(1, D)
    shared["w_in"] = f(inp["w_in"])
    shared["conv_a"] = f(inp["conv_a"]).reshape(2, 5, 24, 128).reshape(2, 120, 128)
    gc = np.zeros((2, 2, 32), np.float32)
    dtb = f(inp["dt_bias"]); alog = f(inp["a_log"])
    for d in range(2):
        gc[:, 0, d * 16:d * 16 + 8] = dtb[:, d, :]
        gc[:, 1, d * 16:d * 16 + 8] = alog[:, d, :]
    shared["gdn_cols"] = gc
    shared["gdn_norm"] = f(inp["gdn_norm"]).reshape(2, 1, 128)
    shared["sink_b"] = f(inp["sink_b"]).reshape(2, 1, 8)
    rp = np.zeros((2, 8, 15, 128), np.float32)
    rp[:, :, :, 48:79] = f(inp["rpb_c"])[..., ::-1]
    shared["rpb_pad"] = rp.reshape(2, 120, 128)
    shared["w_out"] = f(inp["w_out"])
    shared["w_r"] = np.ascontiguousarray(np.concatenate([f(inp["w_router_group"]), f(inp["w_router_expert"])], -1))
    shared["b_r"] = np.ascontiguousarray(np.concatenate([f(inp["b_router_group"]), f(inp["b_router_expert"])], -1)).reshape(2, 1, 36)
    if need_moe:
        shared["w_gate"] = f(inp["w_gate"])
        shared["w_up"] = f(inp["w_up"])
        shared["w_down"] = f(inp["w_down"])
    shared["cst"] = make_consts()
    rc, rs = rope_tables()
    shared["ropec"] = rc
    shared["ropes"] = rs
    x = f(inp["x"]); ctx = f(inp["ctx"]); c = f(inp["c"]); cc = f(inp["c_ctx"])
    maps = []
    for b in range(ncores):
        m = dict(shared)
        m["x"] = x[b]
        m["ctx"] = ctx[b]
        m["cvec"] = np.ascontiguousarray(np.concatenate([c[b].reshape(16, 128), cc.reshape(16, 128)], 0))
        maps.append(m)
    return maps


_NC_CACHE = {}


def kernel(**inputs):
    if "nc" not in _NC_CACHE:
        _NC_CACHE["nc"] = build().nc
    maps = make_in_maps(inputs)
    res = run_bass_kernel_spmd(_NC_CACHE["nc"], maps, core_ids=list(range(8)))
    return np.stack([np.asarray(r["out"], dtype=np.float32) for r in res.results], 0)
```
